# Optimizing a Trainium2 kernel written in Bass

```python
import math
import jax, jax.numpy as jnp
from jax import lax
import numpy as np

D_MODEL = 2048
BATCH = 4
SEQ = 4096
DEPTH = 4

GRID_W = 64
CTX_LEN = 256
HEAD_DIM = 128
DIFF_HEADS = 4
GQA_HEADS = 8
GQA_KV_HEADS = 2
GQA_GROUP = GQA_HEADS // GQA_KV_HEADS
DIFF_Q = DIFF_HEADS * 2 * HEAD_DIM
DIFF_V = DIFF_HEADS * 2 * HEAD_DIM
GQA_Q = GQA_HEADS * HEAD_DIM
GQA_KV = GQA_KV_HEADS * HEAD_DIM
ATTN_IN = 3 * DIFF_Q + GQA_Q + 2 * GQA_KV
ATTN_OUT = DIFF_V + GQA_Q
ATTN_SPLITS = (DIFF_Q, 2 * DIFF_Q, 2 * DIFF_Q + DIFF_V, 2 * DIFF_Q + DIFF_V + GQA_Q, 2 * DIFF_Q + DIFF_V + GQA_Q + GQA_KV)
Q_BLOCK = 128
ROPE_THETA = 10000.0
ROPE_PAIRS_AXIS = HEAD_DIM // 4
QK_NORM_EPS = 1e-6
SUBLN_EPS = 1e-5
HYENA_EMB = 33
HYENA_BANDS = (HYENA_EMB - 1) // 2
HYENA_FW = 64
HYENA_TARGET = 1e-2
HYENA_MIN_DECAY = math.log(HYENA_TARGET) / 1.5
HYENA_MAX_DECAY = math.log(HYENA_TARGET) / 0.3
N_EXPERTS = 32
TOP_K = 4
D_EXPERT = 768
SWIGLU_LIMIT = 7.0
SWIGLU_ALPHA = 1.702
MOE_BLOCK = 128
N_EVEN = (DEPTH + 1) // 2
N_ODD = DEPTH // 2
DEEPNORM_ALPHA = (2 * DEPTH) ** 0.25
DEEPNORM_BETA = (8 * DEPTH) ** -0.25
LN_EPS = 1e-5

kernel_name = "hybrid_diffattn_gqa_hyena_moe_deepnorm"


def _layer_norm(x, g, b):
    xf = x.astype(jnp.float32)
    mu = jnp.mean(xf, -1, keepdims=True)
    var = jnp.mean(jnp.square(xf - mu), -1, keepdims=True)
    return ((xf - mu) * lax.rsqrt(var + LN_EPS) * g + b).astype(x.dtype)


def _rms_norm(x, w, eps):
    xf = x.astype(jnp.float32)
    return (xf * lax.rsqrt(jnp.mean(xf * xf, -1, keepdims=True) + eps) * w).astype(x.dtype)


def _grid_rope_tables(n_tokens):
    rows = n_tokens // GRID_W
    row = jnp.broadcast_to(jnp.arange(rows, dtype=jnp.float32)[:, None], (rows, GRID_W)).reshape(-1)
    col = jnp.broadcast_to(jnp.arange(GRID_W, dtype=jnp.float32)[None, :], (rows, GRID_W)).reshape(-1)
    inv = ROPE_THETA ** (-jnp.arange(ROPE_PAIRS_AXIS, dtype=jnp.float32) / ROPE_PAIRS_AXIS)
    ang = jnp.concatenate([row[:, None] * inv, col[:, None] * inv], axis=-1)
    return jnp.cos(ang)[:, None, :], jnp.sin(ang)[:, None, :]


def _rope(x, cos, sin):
    xf = x.astype(jnp.float32)
    x1, x2 = xf[..., :HEAD_DIM // 2], xf[..., HEAD_DIM // 2:]
    return jnp.concatenate([x1 * cos - x2 * sin, x2 * cos + x1 * sin], axis=-1).astype(x.dtype)


def _sweep_query_blocks(attend, q):
    *lead, n, d = q.shape
    nb = n // Q_BLOCK
    qb = jnp.moveaxis(q.reshape(*lead, nb, Q_BLOCK, d), -3, 0)
    ob = jnp.moveaxis(lax.map(attend, qb), 0, -3)
    return ob.reshape(*lead, n, ob.shape[-1])


def _diff_attend(q, k, v, lam):
    q = q.reshape(*q.shape[:-1], 2, HEAD_DIM)
    s = jnp.einsum('bhqmd,bhtmd->bhmqt', q, k).astype(jnp.float32) * HEAD_DIM ** -0.5
    p = jax.nn.softmax(s, axis=-1)
    p = p[:, :, 0] - lam * p[:, :, 1]
    return jnp.einsum('bhqt,bhtd->bhqd', p.astype(v.dtype), v)


def _gqa_attend(q, k, v):
    s = jnp.einsum('bkgqd,bktd->bkgqt', q, k).astype(jnp.float32) * HEAD_DIM ** -0.5
    p = jax.nn.softmax(s, axis=-1)
    return jnp.einsum('bkgqt,bktd->bkgqd', p.astype(v.dtype), v)


def _attn_mixer(hc, hl, w_in, w_out, lq1, lk1, lq2, lk2, subln_w, qn_w, kn_w, lam_init, need_ctx):
    lam = (jnp.exp(jnp.sum((lq1 * lk1).astype(jnp.float32))) - jnp.exp(jnp.sum((lq2 * lk2).astype(jnp.float32))) + lam_init)
    cos, sin = _grid_rope_tables(hl.shape[1])

    def project(h, rotate):
        b, n, _ = h.shape
        qa, ka, va, qb, kb, vb = jnp.split(h @ w_in, ATTN_SPLITS, axis=-1)
        qa = qa.reshape(b, n, 2 * DIFF_HEADS, HEAD_DIM)
        ka = ka.reshape(b, n, 2 * DIFF_HEADS, HEAD_DIM)
        qb = _rms_norm(qb.reshape(b, n, GQA_HEADS, HEAD_DIM), qn_w, QK_NORM_EPS)
        kb = _rms_norm(kb.reshape(b, n, GQA_KV_HEADS, HEAD_DIM), kn_w, QK_NORM_EPS)
        if rotate:
            qa, ka, qb, kb = _rope(qa, cos, sin), _rope(ka, cos, sin), _rope(qb, cos, sin), _rope(kb, cos, sin)
        qa = qa.reshape(b, n, DIFF_HEADS, 2 * HEAD_DIM).transpose(0, 2, 1, 3)
        ka = ka.reshape(b, n, DIFF_HEADS, 2 * HEAD_DIM).transpose(0, 2, 1, 3)
        va = va.reshape(b, n, DIFF_HEADS, 2 * HEAD_DIM).transpose(0, 2, 1, 3)
        qb = qb.reshape(b, n, GQA_KV_HEADS, GQA_GROUP, HEAD_DIM).transpose(0, 2, 3, 1, 4)
        kb = kb.transpose(0, 2, 1, 3)
        vb = vb.reshape(b, n, GQA_KV_HEADS, HEAD_DIM).transpose(0, 2, 1, 3)
        return qa, ka, va, qb, kb, vb

    def merge(oa, ob):
        b, _, n, _ = oa.shape
        oa = _rms_norm(oa, subln_w, SUBLN_EPS) * (1.0 - lam_init)
        oa = oa.transpose(0, 2, 1, 3).reshape(b, n, DIFF_V)
        ob = ob.transpose(0, 3, 1, 2, 4).reshape(b, n, GQA_Q)
        return jnp.concatenate([oa, ob], axis=-1) @ w_out

    qa_c, ka_c, va_c, qb_c, kb_c, vb_c = project(hc, False)
    qa_l, ka_l, va_l, qb_l, kb_l, vb_l = project(hl, True)
    b, n_c = hc.shape[:2]
    ka_c2 = ka_c.reshape(b, DIFF_HEADS, n_c, 2, HEAD_DIM)
    ka_all = jnp.concatenate([ka_c, ka_l], axis=2)
    ka_all = ka_all.reshape(b, DIFF_HEADS, ka_all.shape[2], 2, HEAD_DIM)
    va_all = jnp.concatenate([va_c, va_l], axis=2)
    kb_all = jnp.concatenate([kb_c, kb_l], axis=2)
    vb_all = jnp.concatenate([vb_c, vb_l], axis=2)
    oa_l = _sweep_query_blocks(lambda q: _diff_attend(q, ka_all, va_all, lam), qa_l)
    ob_l = _sweep_query_blocks(lambda q: _gqa_attend(q, kb_all, vb_all), qb_l)
    out_l = merge(oa_l, ob_l)
    if need_ctx:
        out_c = merge(_diff_attend(qa_c, ka_c2, va_c, lam), _gqa_attend(qb_c, kb_c, vb_c))
    else:
        out_c = None
    return out_c, out_l


def _hyena_filter(n, w1, b1, w2, b2, w3, b3, freq, w4):
    f32 = jnp.float32
    t = jnp.linspace(0.0, 1.0, n, dtype=f32)[:, None]
    w = 2.0 * math.pi * jnp.arange(n, dtype=f32)[:, None] / n
    f = jnp.linspace(1e-4, HYENA_BANDS - 1, HYENA_BANDS, dtype=f32)[None, :]
    z = jnp.concatenate([t, jnp.cos(f * w), -jnp.sin(f * w)], axis=-1)
    fr = freq.astype(f32)
    a = jnp.sin(fr * (z @ w1.astype(f32) + b1.astype(f32)))
    a = jnp.sin(fr * (a @ w2.astype(f32) + b2.astype(f32)))
    a = jnp.sin(fr * (a @ w3.astype(f32) + b3.astype(f32)))
    hk = a @ w4.astype(f32)
    deltas = jnp.abs(jnp.linspace(HYENA_MIN_DECAY, HYENA_MAX_DECAY, D_MODEL, dtype=f32))
    decay = jnp.exp(-t * deltas)
    h_fwd = hk[:, :D_MODEL] * decay
    h_bwd = hk[:, D_MODEL:] * decay
    return jnp.concatenate([h_fwd, jnp.zeros((1, D_MODEL), f32), h_bwd[:0:-1]], axis=0)


def _short_conv(u, w, b):
    up = jnp.pad(u, ((0, 0), (1, 1), (0, 0)))
    return up[:, :-2] * w[0] + up[:, 1:-1] * w[1] + up[:, 2:] * w[2] + b


def _hyena(h, w_in, b_in, conv_w, conv_b, f_w1, f_b1, f_w2, f_b2, f_w3, f_b3, f_freq, f_w4, long_bias, w_out, b_out):
    n = h.shape[1]
    u = _short_conv(h @ w_in + b_in, conv_w, conv_b)
    x0, x1, v = jnp.split(u, 3, axis=-1)
    kern = _hyena_filter(n, f_w1, f_b1, f_w2, f_b2, f_w3, f_b3, f_freq, f_w4)
    z = (v * x1).astype(jnp.float32)
    y = jnp.fft.irfft(jnp.fft.rfft(z, n=2 * n, axis=1) * jnp.fft.rfft(kern, axis=0)[None], n=2 * n, axis=1)[:, :n]
    y = (y + z * long_bias.astype(jnp.float32)).astype(h.dtype) * x0
    return y @ w_out + b_out


def _clamped_swiglu(u):
    glu, lin = u[..., :D_EXPERT], u[..., D_EXPERT:]
    glu = jnp.minimum(glu, SWIGLU_LIMIT)
    lin = jnp.clip(lin, -SWIGLU_LIMIT, SWIGLU_LIMIT)
    return glu * jax.nn.sigmoid(SWIGLU_ALPHA * glu) * (lin + 1.0)


def _moe(h, w_router, b_router, w_up, b_up, w_down, b_down):
    n_tok, d = h.shape
    logits = (h @ w_router).astype(jnp.float32) + b_router.astype(jnp.float32)
    top_logit, top_idx = lax.top_k(logits, TOP_K)
    gate = jax.nn.softmax(top_logit, axis=-1).reshape(-1)
    flat_e = top_idx.reshape(-1).astype(jnp.int32)
    n_assign = n_tok * TOP_K
    order = jnp.argsort(flat_e).astype(jnp.int32)
    sorted_e = flat_e[order]
    counts = jnp.zeros((N_EXPERTS,), jnp.int32).at[flat_e].add(1)
    padded = (counts + MOE_BLOCK - 1) // MOE_BLOCK * MOE_BLOCK
    pad_end = jnp.cumsum(padded)
    pad_start = pad_end - padded
    start = jnp.cumsum(counts) - counts
    dest = pad_start[sorted_e] + jnp.arange(n_assign, dtype=jnp.int32) - start[sorted_e]
    n_blocks = -(-(n_assign + N_EXPERTS * (MOE_BLOCK - 1)) // MOE_BLOCK)
    n_slots = n_blocks * MOE_BLOCK
    slot_tok = jnp.full((n_slots,), n_tok, jnp.int32).at[dest].set(order // TOP_K)
    slot_gate = jnp.zeros((n_slots,), jnp.float32).at[dest].set(gate[order])
    block_e = jnp.minimum(jnp.searchsorted(pad_end, jnp.arange(n_blocks, dtype=jnp.int32) * MOE_BLOCK, side='right'), N_EXPERTS - 1)
    h_pad = jnp.concatenate([h, jnp.zeros((1, d), h.dtype)], axis=0)

    def expert_block(args):
        tok, g, e = args
        u = h_pad[tok] @ w_up[e] + b_up[e]
        return (_clamped_swiglu(u) @ w_down[e] + b_down[e]) * g[:, None].astype(h.dtype)

    ys = lax.map(expert_block, (slot_tok.reshape(n_blocks, MOE_BLOCK), slot_gate.reshape(n_blocks, MOE_BLOCK), block_e))
    return jnp.zeros_like(h_pad).at[slot_tok].add(ys.reshape(n_slots, d))[:n_tok]


def setup_inputs(seed: int = 0) -> dict:
    key = jax.random.key(seed)
    ks = iter(jax.random.split(key, 64))

    def nrm(shape, scale):
        return scale * jax.random.normal(next(ks), shape, jnp.float32)

    D = D_MODEL
    inp = {}
    inp["x"] = nrm((BATCH, SEQ, D), 1.0)
    inp["c"] = nrm((BATCH, D), 1.0)
    inp["ctx"] = nrm((BATCH, CTX_LEN, D), 1.0)
    inp["c_ctx"] = nrm((D,), 1.0)
    inp["ada_w"] = nrm((DEPTH, D, 6 * D), 0.5 * D ** -0.5)
    inp["ada_b"] = nrm((DEPTH, 6 * D), 0.02)
    inp["ln1_g"] = 1.0 + nrm((DEPTH, D), 0.05)
    inp["ln1_b"] = nrm((DEPTH, D), 0.02)
    inp["ln2_g"] = 1.0 + nrm((DEPTH, D), 0.05)
    inp["ln2_b"] = nrm((DEPTH, D), 0.02)
    inp["attn_w_in"] = nrm((N_EVEN, D, ATTN_IN), D ** -0.5)
    inp["attn_w_out"] = nrm((N_EVEN, ATTN_OUT, D), ATTN_OUT ** -0.5 * DEEPNORM_BETA)
    inp["diff_lam_q1"] = nrm((N_EVEN, HEAD_DIM), 0.1)
    inp["diff_lam_k1"] = nrm((N_EVEN, HEAD_DIM), 0.1)
    inp["diff_lam_q2"] = nrm((N_EVEN, HEAD_DIM), 0.1)
    inp["diff_lam_k2"] = nrm((N_EVEN, HEAD_DIM), 0.1)
    inp["diff_subln_w"] = 1.0 + nrm((N_EVEN, 2 * HEAD_DIM), 0.05)
    inp["gqa_q_norm_w"] = 1.0 + nrm((N_EVEN, HEAD_DIM), 0.05)
    inp["gqa_k_norm_w"] = 1.0 + nrm((N_EVEN, HEAD_DIM), 0.05)
    inp["hy_w_in"] = nrm((N_ODD, D, 3 * D), D ** -0.5)
    inp["hy_b_in"] = nrm((N_ODD, 3 * D), 0.02)
    inp["hy_conv_w"] = nrm((N_ODD, 3, 3 * D), 3 ** -0.5)
    inp["hy_conv_b"] = nrm((N_ODD, 3 * D), 0.02)
    inp["hy_f_w1"] = nrm((N_ODD, HYENA_EMB, HYENA_FW), HYENA_EMB ** -0.5)
    inp["hy_f_b1"] = nrm((N_ODD, HYENA_FW), 0.1)
    inp["hy_f_w2"] = nrm((N_ODD, HYENA_FW, HYENA_FW), HYENA_FW ** -0.5)
    inp["hy_f_b2"] = nrm((N_ODD, HYENA_FW), 0.1)
    inp["hy_f_w3"] = nrm((N_ODD, HYENA_FW, HYENA_FW), HYENA_FW ** -0.5)
    inp["hy_f_b3"] = nrm((N_ODD, HYENA_FW), 0.1)
    inp["hy_f_freq"] = 1.0 + nrm((N_ODD, HYENA_FW), 0.05)
    inp["hy_f_w4"] = nrm((N_ODD, HYENA_FW, 2 * D), 0.005)
    inp["hy_long_bias"] = nrm((N_ODD, D), 0.5)
    inp["hy_w_out"] = nrm((N_ODD, D, D), D ** -0.5 * DEEPNORM_BETA)
    inp["hy_b_out"] = nrm((N_ODD, D), 0.02)
    inp["moe_w_router"] = nrm((DEPTH, D, N_EXPERTS), D ** -0.5)
    inp["moe_b_router"] = nrm((DEPTH, N_EXPERTS), 0.01)
    inp["moe_w_up"] = nrm((DEPTH, N_EXPERTS, D, 2 * D_EXPERT), D ** -0.5)
    inp["moe_b_up"] = nrm((DEPTH, N_EXPERTS, 2 * D_EXPERT), 0.02)
    inp["moe_w_down"] = nrm((DEPTH, N_EXPERTS, D_EXPERT, D), D_EXPERT ** -0.5 * DEEPNORM_BETA)
    inp["moe_b_down"] = nrm((DEPTH, N_EXPERTS, D), 0.02)
    return inp


def reference(x, c, ctx, c_ctx, ada_w, ada_b, ln1_g, ln1_b, ln2_g, ln2_b,
              attn_w_in, attn_w_out, diff_lam_q1, diff_lam_k1, diff_lam_q2, diff_lam_k2, diff_subln_w,
              gqa_q_norm_w, gqa_k_norm_w,
              hy_w_in, hy_b_in, hy_conv_w, hy_conv_b, hy_f_w1, hy_f_b1, hy_f_w2, hy_f_b2, hy_f_w3, hy_f_b3,
              hy_f_freq, hy_f_w4, hy_long_bias, hy_w_out, hy_b_out,
              moe_w_router, moe_b_router, moe_w_up, moe_b_up, moe_w_down, moe_b_down):
    b, s, d = x.shape
    n_c = ctx.shape[1]
    xl, xc = x, ctx
    cond_l = jax.nn.silu(c)
    cond_c = jax.nn.silu(c_ctx)
    for l in range(DEPTH):
        last = l == DEPTH - 1
        even = l % 2 == 0
        ctx_in = (not last) or even
        i = l // 2
        sh1_l, sc1_l, g1_l, sh2_l, sc2_l, g2_l = [m[:, None, :] for m in jnp.split(cond_l @ ada_w[l] + ada_b[l], 6, axis=-1)]
        if ctx_in:
            sh1_c, sc1_c, g1_c, sh2_c, sc2_c, g2_c = jnp.split(cond_c @ ada_w[l] + ada_b[l], 6, axis=-1)
            hc = xc * (1.0 + sc1_c) + sh1_c
        hl = xl * (1.0 + sc1_l) + sh1_l
        if even:
            lam_init = 0.8 - 0.6 * math.exp(-0.3 * l)
            oc, ol = _attn_mixer(hc, hl, attn_w_in[i], attn_w_out[i], diff_lam_q1[i], diff_lam_k1[i],
                                 diff_lam_q2[i], diff_lam_k2[i], diff_subln_w[i], gqa_q_norm_w[i],
                                 gqa_k_norm_w[i], lam_init, not last)
        else:
            hp = (hy_w_in[i], hy_b_in[i], hy_conv_w[i], hy_conv_b[i], hy_f_w1[i], hy_f_b1[i], hy_f_w2[i],
                  hy_f_b2[i], hy_f_w3[i], hy_f_b3[i], hy_f_freq[i], hy_f_w4[i], hy_long_bias[i], hy_w_out[i], hy_b_out[i])
            ol = _hyena(hl, *hp)
            oc = _hyena(hc, *hp) if not last else None
        xl = _layer_norm(DEEPNORM_ALPHA * xl + g1_l * ol, ln1_g[l], ln1_b[l])
        hl = xl * (1.0 + sc2_l) + sh2_l
        moe_p = (moe_w_router[l], moe_b_router[l], moe_w_up[l], moe_b_up[l], moe_w_down[l], moe_b_down[l])
        if last:
            fl = _moe(hl.reshape(b * s, d), *moe_p).reshape(b, s, d)
        else:
            xc = _layer_norm(DEEPNORM_ALPHA * xc + g1_c * oc, ln1_g[l], ln1_b[l])
            hc = xc * (1.0 + sc2_c) + sh2_c
            f = _moe(jnp.concatenate([hc.reshape(b * n_c, d), hl.reshape(b * s, d)], axis=0), *moe_p)
            fc = f[:b * n_c].reshape(b, n_c, d)
            fl = f[b * n_c:].reshape(b, s, d)
            xc = _layer_norm(DEEPNORM_ALPHA * xc + g2_c * fc, ln2_g[l], ln2_b[l])
        xl = _layer_norm(DEEPNORM_ALPHA * xl + g2_l * fl, ln2_g[l], ln2_b[l])
    return xl
```

```python
import math
import numpy as np
from contextlib import ExitStack
import concourse.bass as bass
import concourse.mybir as mybir
from concourse.bass_utils import run_bass_kernel_spmd

F32 = mybir.dt.float32
BF16 = mybir.dt.bfloat16
AF = mybir.ActivationFunctionType
ALU = mybir.AluOpType
AX = mybir.AxisListType

D = 2048
KT = 16
NEXP = 32
DEXP = 768
DEPTH = 4
ALPHA = (2 * DEPTH) ** 0.25
LN_EPS = 1e-5
NCORES = 8
import os
P1_STEPS = int(os.environ.get('P1_STEPS', '99'))


class Sched:
    ENG = ("pe", "act", "dve", "pool", "sp")

    def __init__(self, nc, es, same_engine_sync=True):
        self.nc = nc
        self.es = es
        self.e = {"pe": nc.tensor, "act": nc.scalar, "dve": nc.vector,
                  "pool": nc.gpsimd, "sp": nc.sync}
        self.sem = {k: es.enter_context(nc.semaphore("s_" + k)) for k in self.ENG}
        self.cnt = {k: 0 for k in self.ENG}
        self.seen = {k: {} for k in self.ENG}
        self.semobj = {self.sem[k].name: self.sem[k] for k in self.ENG}
        self.own = {self.sem[k].name: k for k in self.ENG}
        self.last_w = {}
        self.readers = {}
        self.same = same_engine_sync
        self.dpool = {}
        for q, n in (("sp", 32), ("pool", 32), ("act", 8)):
            sems = [es.enter_context(nc.semaphore(f"d_{q}{i}")) for i in range(n)]
            for s in sems:
                self.semobj[s.name] = s
            self.dpool[q] = {"sems": sems, "val": [0] * n, "next": 0}
        self.latest = {}
        self.n_wait = 0
        self.n_ins = 0

    def _wait(self, eng, ev):
        name, val = ev
        if self.own.get(name) == eng and (not self.same or eng == "pe"):
            return
        if self.seen[eng].get(name, 0) >= val:
            return
        self.seen[eng][name] = val
        self.e[eng].wait_ge(self.semobj[name], val)
        self.n_wait += 1

    def _deps(self, eng, r, w):
        evs = []
        for k in r:
            if k in self.last_w:
                evs.append(self.last_w[k])
        for k in w:
            if k in self.last_w:
                evs.append(self.last_w[k])
            evs.extend(self.readers.get(k, ()))
        best = {}
        for name, val in evs:
            if best.get(name, 0) < val:
                best[name] = val
        for name, val in best.items():
            self._wait(eng, (name, val))

    def _record(self, ev, r, w):
        for k in w:
            self.last_w[k] = ev
            self.readers[k] = []
        for k in r:
            self.readers.setdefault(k, []).append(ev)
        self.latest[ev[0]] = max(self.latest.get(ev[0], 0), ev[1])

    def op(self, eng, fn, r=(), w=()):
        self._deps(eng, r, w)
        ins = fn(self.e[eng])
        self.cnt[eng] += 1
        ins.then_inc(self.sem[eng], 1)
        ev = (self.sem[eng].name, self.cnt[eng])
        self._record(ev, r, w)
        self.n_ins += 1
        return ev

    def dma(self, q, out, in_, r=(), w=(), **kw):
        p = self.dpool[q]
        i = p["next"]
        p["next"] = (i + 1) % len(p["sems"])
        s = p["sems"][i]
        if p["val"][i] > 0:
            self._wait(q, (s.name, p["val"][i]))
        self._deps(q, r, w)
        ins = self.e[q].dma_start(out=out, in_=in_, **kw)
        p["val"][i] += 16
        ins.then_inc(s, 16)
        ev = (s.name, p["val"][i])
        self._record(ev, r, w)
        self.n_ins += 1
        return ev

    def wait_all(self, eng):
        for name, val in list(self.latest.items()):
            self._wait(eng, (name, val))

    def barrier(self):
        for eng in self.ENG:
            self.wait_all(eng)
        self.last_w.clear()
        self.readers.clear()

    def _uniq(self, name):
        self._uid = getattr(self, "_uid", 0) + 1
        return f"t{self._uid}_{name}"

    def sb(self, name, shape, dtype, es=None):
        return (es or self.es).enter_context(self.nc.sbuf_tensor(self._uniq(name), list(shape), dtype))

    def ps(self, name, shape, dtype=F32, es=None):
        return (es or self.es).enter_context(self.nc.psum_tensor(self._uniq(name), list(shape), dtype))


def _dram_in(nc, name, shape, dt=F32):
    return nc.dram_tensor(name, list(shape), dt, kind="ExternalInput").ap()


def _layer_norm_tile(S, r, rk, out, ok, gB, bB, scr):
    stats, mv, rstd = scr
    sk = [(stats.name, c) for c in range(4)]
    for c in range(4):
        S.op("dve", lambda e, c=c: e.bn_stats(stats[:, c, :], r[:, c * 512:(c + 1) * 512]),
             r=rk, w=[sk[c]])
    S.op("dve", lambda e: e.bn_aggr(mv[:], stats[:]), r=sk, w=[mv.name])
    S.op("dve", lambda e: e.tensor_scalar(rstd[:], mv[:, 1:2], LN_EPS, None, ALU.add),
         r=[mv.name], w=[rstd.name])
    S.op("act", lambda e: e.activation(out=rstd[:], in_=rstd[:], func=AF.Sqrt), r=[rstd.name], w=[rstd.name])
    S.op("dve", lambda e: e.reciprocal(rstd[:], rstd[:]), r=[rstd.name], w=[rstd.name])
    S.op("dve", lambda e: e.tensor_scalar(out[:], r[:], mv[:, 0:1], rstd[:], ALU.subtract, ALU.mult),
         r=rk + [mv.name, rstd.name], w=ok)
    S.op("pool", lambda e: e.tensor_tensor(out[:], out[:], gB[:], ALU.mult), r=ok + [gB.name], w=ok)
    S.op("pool", lambda e: e.tensor_tensor(out[:], out[:], bB[:], ALU.add), r=ok + [bB.name], w=ok)


def _compute_mod(S, es, condT_d, ada_w_d, ada_b_d, modscr_d, col0, ncols, one_plus_cols, nrows=2):
    nc = S.nc
    with ExitStack() as les:
        cT = S.sb("cT", [128, KT, nrows], F32, les)
        cTb = S.sb("cTb", [128, KT, nrows], BF16, les)
        S.dma("sp", cT[:], condT_d, w=["cT"])
        S.op("act", lambda e: e.activation(out=cTb[:], in_=cT[:], func=AF.Silu), r=["cT"], w=["cTb"])
        wbuf = [S.sb(f"adaw{i}", [128, KT, 512], BF16, les) for i in range(2)]
        brow = S.sb("adab", [nrows, ncols], F32, les)
        mrow = S.sb("mrow", [nrows, ncols], F32, les)
        pm = S.ps("pm", [nrows, 512], F32, les)
        for rr in range(nrows):
            S.dma("sp", brow[rr:rr + 1, :], ada_b_d[0:1, col0:col0 + ncols], w=[("adab", rr)])
        awv = ada_w_d.rearrange("(kt p) n -> p kt n", p=128)
        for g in range(ncols // 512):
            wb = wbuf[g % 2]
            S.dma("pool", wb[:], awv[:, :, col0 + g * 512: col0 + (g + 1) * 512], w=[wb.name])
            for kt in range(KT):
                S.op("pe", lambda e, kt=kt, wb=wb: e.matmul(pm[:], cTb[:, kt, :], wb[:, kt, :],
                                                             start=(kt == 0), stop=(kt == KT - 1)),
                     r=["cTb", wb.name] if kt else ["cTb", wb.name], w=["pm"] if kt in (0, KT - 1) else [])
            S.op("dve", lambda e, g=g: e.tensor_tensor(mrow[:, g * 512:(g + 1) * 512], pm[:],
                                                         brow[:, g * 512:(g + 1) * 512], ALU.add),
                 r=["pm"] + [("adab", rr) for rr in range(nrows)], w=[("mrow", g)])
        for (a, b) in one_plus_cols:
            S.op("dve", lambda e, a=a, b=b: e.tensor_scalar(mrow[:, a:b], mrow[:, a:b], 1.0, None, ALU.add),
                 r=[("mrow", g) for g in range(ncols // 512)], w=[("mrow", g) for g in range(ncols // 512)])
        S.dma("sp", modscr_d, mrow[:], r=[("mrow", g) for g in range(ncols // 512)], w=["modscr"])
        S.barrier()


def build_tail(NT, has_ctx_tile, split, stop_after=None, debug=False, gated=False):
    NTT = NT // 128
    nc = bass.Bass("TRN2", target_bir_lowering=False)
    m_d = _dram_in(nc, "m", [NT, D])
    x_d = _dram_in(nc, "x", [NT, D])
    m2_d = _dram_in(nc, "m2", [NT, D]) if gated else None
    wout_d = _dram_in(nc, "wout", [D, D])
    bout_d = _dram_in(nc, "bout", [1, D])
    condT_d = _dram_in(nc, "condT", [128, KT, 2])
    ada_w_d = _dram_in(nc, "ada_w", [D, 6 * D])
    ada_b_d = _dram_in(nc, "ada_b", [1, 6 * D])
    ln_d = _dram_in(nc, "ln", [4, D])
    wr_d = _dram_in(nc, "w_router", [D, NEXP])
    br_d = _dram_in(nc, "b_router", [1, NEXP])
    wup_d = _dram_in(nc, "w_up", [NEXP, D, 2 * DEXP])
    bupT_d = _dram_in(nc, "b_upT", [128, NEXP * 12])
    wdn_d = _dram_in(nc, "w_down", [NEXP, DEXP, D])
    bdn_d = _dram_in(nc, "b_down", [NEXP, D])
    ident_d = _dram_in(nc, "ident", [128, 128])
    out_d = nc.dram_tensor("xo", [NT, D], F32, kind="ExternalOutput").ap()
    dk = "ExternalOutput" if debug else "Internal"
    modscr = nc.dram_tensor("modscr", [2, 4 * D], F32, kind=dk).ap()
    x1scr = nc.dram_tensor("x1scr", [NT, D], F32, kind=dk).ap()
    gates_dbg = nc.dram_tensor("gates_dbg", [128, NT // 128, NEXP], F32, kind=dk).ap()
    xtscr = nc.dram_tensor("xtscr", [128, KT, NT], BF16, kind="Internal").ap()

    with ExitStack() as es:
        S = Sched(nc, es)
        ident = S.sb("ident", [128, 128], F32)
        S.dma("sp", ident[:], ident_d, w=["ident"])
        gates = S.sb("gates", [128, NTT, NEXP], F32)
        _compute_mod(S, es, condT_d, ada_w_d, ada_b_d, modscr, 2 * D, 4 * D, [(2 * D, 3 * D)])
        MOD_G1, MOD_SH2, MOD_SC2, MOD_G2 = 0, 1, 2, 3
        if stop_after == "mod":
            return nc

        def bload(q, tile, row_ap, key):
            S.dma(q, tile[:], row_ap.partition_broadcast(128), r=["modscr"], w=[key])

        with ExitStack() as p1:
            wout = S.sb("woutb", [128, KT, D], BF16, p1)
            wov = wout_d.rearrange("(kt p) n -> p kt n", p=128)
            for kt in range(KT):
                S.dma("pool", wout[:, kt, :], wov[:, kt, :], w=[("wout", kt)])
            wr = S.sb("wr", [128, KT, NEXP], F32, p1)
            S.dma("sp", wr[:], wr_d.rearrange("(kt p) n -> p kt n", p=128), w=["wr"])
            brB = S.sb("brB", [128, NEXP], F32, p1)
            S.dma("sp", brB[:], br_d.partition_broadcast(128), w=["brB"])
            g1B = S.sb("g1B", [128, D], F32, p1)
            gbB = S.sb("gbB", [128, D], F32, p1)
            sc2B = S.sb("sc2B", [128, D], F32, p1)
            sh2B = S.sb("sh2B", [128, D], F32, p1)
            lngB = S.sb("lngB", [128, D], F32, p1)
            lnbB = S.sb("lnbB", [128, D], F32, p1)
            S.dma("sp", lngB[:], ln_d[0:1, :].partition_broadcast(128), w=["lngB"])
            S.dma("sp", lnbB[:], ln_d[1:2, :].partition_broadcast(128), w=["lnbB"])
            mt = S.sb("mt", [128, D], F32, p1)
            xt = S.sb("xt", [128, D], F32, p1)
            mT = S.sb("mT", [128, KT, 128], BF16, p1)
            tt_ = S.sb("tt", [128, D], F32, p1)
            x1 = S.sb("x1", [128, D], F32, p1)
            hT = S.sb("hT", [128, KT, 128], F32, p1)
            hTb = S.sb("hTb", [128, KT, 128], BF16, p1)
            stats = S.sb("stats", [128, 4, 6], F32, p1)
            mv = S.sb("mv", [128, 2], F32, p1)
            rstd = S.sb("rstd", [128, 1], F32, p1)
            lg = S.sb("lg", [128, NEXP], F32, p1)
            mx8 = S.sb("mx8", [128, 8], F32, p1)
            nmx = S.sb("nmx", [128, 1], F32, p1)
            msk = S.sb("msk", [128, NEXP], F32, p1)
            ex = S.sb("ex", [128, NEXP], F32, p1)
            sm = S.sb("sm", [128, 1], F32, p1)
            pt = [S.ps(f"pt{i}", [128, 512], F32, p1) for i in range(2)]
            po = [S.ps(f"po{i}", [128, 512], F32, p1) for i in range(2)]
            pl = S.ps("pl", [128, NEXP], F32, p1)

            def load_mod(r):
                bload("sp", g1B, modscr[r:r + 1, MOD_G1 * D:(MOD_G1 + 1) * D], "g1B")
                bload("sp", sc2B, modscr[r:r + 1, MOD_SC2 * D:(MOD_SC2 + 1) * D], "sc2B")
                bload("sp", sh2B, modscr[r:r + 1, MOD_SH2 * D:(MOD_SH2 + 1) * D], "sh2B")
                S.dma("sp", gbB[:], bout_d.partition_broadcast(128), w=["gbB"])
                S.op("dve", lambda e: e.tensor_tensor(gbB[:], gbB[:], g1B[:], ALU.mult), r=["gbB", "g1B"], w=["gbB"])

            load_mod(0)
            tcount = 0
            for i in range(NTT):
                if has_ctx_tile and i == NTT - 1:
                    load_mod(1)
                rows = slice(i * 128, (i + 1) * 128)
                S.dma("sp", mt[:], m_d[rows, :], w=["mt"])
                if gated:
                    S.dma("sp", x1[:], m2_d[rows, :], w=["x1"])
                    S.op("pool", lambda e: e.tensor_tensor(mt[:], mt[:], x1[:], ALU.mult), r=["mt", "x1"], w=["mt"])
                S.dma("sp", xt[:], x_d[rows, :], w=["xt"])
                for q4 in range(4):
                    p_ = pt[tcount % 2]; tcount += 1
                    for j in range(4):
                        kt = q4 * 4 + j
                        S.op("pe", lambda e, p_=p_, j=j, kt=kt: e.transpose(p_[:, j * 128:(j + 1) * 128], mt[:, kt * 128:(kt + 1) * 128], ident[:]),
                             r=["mt", "ident"], w=[p_.name])
                    S.op("act", lambda e, p_=p_, q4=q4: e.activation(out=mT[:, q4 * 4:(q4 + 1) * 4, :], in_=p_[:].rearrange("p (a b) -> p a b", a=4), func=AF.Copy),
                         r=[p_.name], w=[("mT", q4)])
                if P1_STEPS < 20:
                    continue
                for dg in range(4):
                    p_ = po[dg % 2]
                    for kt in range(KT):
                        S.op("pe", lambda e, p_=p_, kt=kt, dg=dg: e.matmul(p_[:], mT[:, kt, :], wout[:, kt, dg * 512:(dg + 1) * 512], start=(kt == 0), stop=(kt == KT - 1)),
                             r=[("mT", kt // 4), ("wout", kt)], w=[p_.name] if kt in (0, KT - 1) else [])
                    S.op("dve", lambda e, p_=p_, dg=dg: e.tensor_tensor(tt_[:, dg * 512:(dg + 1) * 512], p_[:], g1B[:, dg * 512:(dg + 1) * 512], ALU.mult),
                         r=[p_.name, "g1B"], w=[("tt", dg)])
                ttk = [("tt", dg) for dg in range(4)]
                S.op("pool", lambda e: e.tensor_tensor(tt_[:], tt_[:], gbB[:], ALU.add), r=ttk + ["gbB"], w=ttk)
                S.op("dve", lambda e: e.scalar_tensor_tensor(tt_[:], xt[:], ALPHA, tt_[:], ALU.mult, ALU.add), r=ttk + ["xt"], w=ttk)
                if P1_STEPS < 30:
                    continue
                _layer_norm_tile(S, tt_, ttk, x1, ["x1"], lngB, lnbB, (stats, mv, rstd))
                S.dma("sp", x1scr[rows, :], x1[:], r=["x1"], w=[("x1scr", i)])
                if P1_STEPS < 40:
                    continue
                S.op("dve", lambda e: e.tensor_tensor(tt_[:], x1[:], sc2B[:], ALU.mult), r=["x1", "sc2B"], w=ttk)
                S.op("pool", lambda e: e.tensor_tensor(tt_[:], tt_[:], sh2B[:], ALU.add), r=ttk + ["sh2B"], w=ttk)
                for q4 in range(4):
                    p_ = pt[tcount % 2]; tcount += 1
                    for j in range(4):
                        kt = q4 * 4 + j
                        S.op("pe", lambda e, p_=p_, j=j, kt=kt: e.transpose(p_[:, j * 128:(j + 1) * 128], tt_[:, kt * 128:(kt + 1) * 128], ident[:]),
                             r=ttk + ["ident"], w=[p_.name])
                    S.op("act", lambda e, p_=p_, q4=q4: e.activation(out=hT[:, q4 * 4:(q4 + 1) * 4, :], in_=p_[:].rearrange("p (a b) -> p a b", a=4), func=AF.Copy),
                         r=[p_.name], w=[("hT", q4)])
                    if P1_STEPS >= 42:
                        S.op("dve", lambda e, q4=q4: e.tensor_copy(hTb[:, q4 * 4:(q4 + 1) * 4, :], hT[:, q4 * 4:(q4 + 1) * 4, :]),
                             r=[("hT", q4)], w=[("hTb", q4)])
                if P1_STEPS >= 43:
                    S.dma("sp", xtscr[:, :, rows], hTb[:], r=[("hTb", q) for q in range(4)], w=[("xtscr", i)])
                if P1_STEPS < 50:
                    continue
                for kt in range(KT):
                    S.op("pe", lambda e, kt=kt: e.matmul(pl[:], hT[:, kt, :], wr[:, kt, :], start=(kt == 0), stop=(kt == KT - 1)),
                         r=[("hT", kt // 4), "wr"], w=["pl"] if kt in (0, KT - 1) else [])
                S.op("dve", lambda e: e.tensor_tensor(lg[:], pl[:], brB[:], ALU.add), r=["pl", "brB"], w=["lg"])
                S.op("dve", lambda e: e.max(out=mx8[:], in_=lg[:]), r=["lg"], w=["mx8"])
                S.op("dve", lambda e: e.tensor_scalar(msk[:], lg[:], mx8[:, 3:4], None, ALU.is_ge), r=["lg", "mx8"], w=["msk"])
                if P1_STEPS < 60:
                    continue
                S.op("dve", lambda e: e.tensor_scalar(nmx[:], mx8[:, 0:1], -1.0, None, ALU.mult), r=["mx8"], w=["nmx"])
                S.op("act", lambda e: e.activation(out=ex[:], in_=lg[:], func=AF.Exp, bias=nmx[:], scale=1.0), r=["lg", "nmx"], w=["ex"])
                S.op("dve", lambda e: e.tensor_tensor(ex[:], ex[:], msk[:], ALU.mult), r=["ex", "msk"], w=["ex"])
                S.op("dve", lambda e: e.reduce_sum(out=sm[:], in_=ex[:], axis=AX.X), r=["ex"], w=["sm"])
                S.op("dve", lambda e: e.reciprocal(sm[:], sm[:]), r=["sm"], w=["sm"])
                S.op("dve", lambda e, i=i: e.tensor_scalar(gates[:, i, :], ex[:], sm[:], None, ALU.mult), r=["ex", "sm"], w=[("gates", i)])
            S.barrier()
            if debug:
                S.dma("sp", gates_dbg, gates[:], w=["gdbg"])
                S.barrier()
        if stop_after == "p1":
            return nc

        bupT = S.sb("bupT", [128, NEXP * 12], F32)
        S.dma("sp", bupT[:], bupT_d, w=["bupT"])
        ones = S.sb("ones", [1, 128], BF16)
        S.op("dve", lambda e: e.memset(ones[:], 1.0), w=["ones"])
        t0 = 0
        for (ntl, groups) in split:
            nh = ntl * 128
            tok0 = t0 * 128
            with ExitStack() as p2:
                Y = S.sb("Y", [128, ntl, D], F32, p2)
                with ExitStack() as p2b:
                    XT = S.sb("XT", [128, KT, nh], BF16, p2b)
                    for kt in range(KT):
                        S.dma("sp", XT[:, kt, :], xtscr[:, kt, tok0:tok0 + nh], r=[("xtscr", i) for i in range(NTT)], w=[("XT", kt)])
                    AT = [S.sb(f"AT{i}", [128, 6, nh], BF16, p2b) for i in range(2)]
                    NW = 3
                    wug = [S.sb(f"wug{i}", [128, KT, 128], BF16, p2b) for i in range(NW)]
                    wul = [S.sb(f"wul{i}", [128, KT, 128], BF16, p2b) for i in range(NW)]
                    wd = [S.sb(f"wd{i}", [128, 6, 512], BF16, p2b) for i in range(2)]
                    bd = [S.sb(f"bd{i}", [1, D], BF16, p2b) for i in range(2)]
                    gmax = max(groups)
                    gt = [S.sb(f"gt{i}", [128, gmax], F32, p2b) for i in range(2)]
                    st = [S.sb(f"st{i}", [128, gmax], F32, p2b) for i in range(2)]
                    lt = [S.sb(f"lt{i}", [128, gmax], F32, p2b) for i in range(2)]
                    pg = [S.ps(f"pg{i}", [128, 512], F32, p2b) for i in range(2)]
                    plin = [S.ps(f"plin{i}", [128, 512], F32, p2b) for i in range(2)]
                    pd = [S.ps(f"pd{i}", [128, 512], F32, p2b) for i in range(3)]
                    cnt = {"w": 0, "g": 0, "d": 0, "pd": 0}

                    def up(e):
                        at = AT[e % 2]
                        wv = wup_d[e].rearrange("(kt p) n -> p kt n", p=128)
                        for jf in range(6):
                            s_ = cnt["w"] % NW; cnt["w"] += 1
                            S.dma("pool", wug[s_][:], wv[:, :, jf * 128:(jf + 1) * 128], w=[wug[s_].name])
                            S.dma("pool", wul[s_][:], wv[:, :, DEXP + jf * 128:DEXP + (jf + 1) * 128], w=[wul[s_].name])
                            c0 = 0
                            for gsz in groups:
                                k_ = cnt["g"] % 2; cnt["g"] += 1
                                for kt in range(KT):
                                    S.op("pe", lambda e_, kt=kt, k_=k_, s_=s_, c0=c0, gsz=gsz: e_.matmul(pg[k_][:, :gsz], wug[s_][:, kt, :], XT[:, kt, c0:c0 + gsz], start=(kt == 0), stop=(kt == KT - 1)),
                                         r=[wug[s_].name, ("XT", kt)], w=[pg[k_].name] if kt in (0, KT - 1) else [])
                                for kt in range(KT):
                                    S.op("pe", lambda e_, kt=kt, k_=k_, s_=s_, c0=c0, gsz=gsz: e_.matmul(plin[k_][:, :gsz], wul[s_][:, kt, :], XT[:, kt, c0:c0 + gsz], start=(kt == 0), stop=(kt == KT - 1)),
                                         r=[wul[s_].name, ("XT", kt)], w=[plin[k_].name] if kt in (0, KT - 1) else [])
                                bg = bupT[:, e * 12 + jf:e * 12 + jf + 1]
                                bl = bupT[:, e * 12 + 6 + jf:e * 12 + 6 + jf + 1]
                                S.op("dve", lambda e_, k_=k_, gsz=gsz, bg=bg: e_.tensor_scalar(gt[k_][:, :gsz], pg[k_][:, :gsz], bg, 7.0, ALU.add, ALU.min),
                                     r=[pg[k_].name, "bupT"], w=[gt[k_].name])
                                S.op("act", lambda e_, k_=k_, gsz=gsz: e_.activation(out=st[k_][:, :gsz], in_=gt[k_][:, :gsz], func=AF.Sigmoid, scale=1.702),
                                     r=[gt[k_].name], w=[st[k_].name])
                                S.op("dve", lambda e_, k_=k_, gsz=gsz, bl=bl: e_.tensor_scalar(lt[k_][:, :gsz], plin[k_][:, :gsz], bl, 7.0, ALU.add, ALU.min),
                                     r=[plin[k_].name, "bupT"], w=[lt[k_].name])
                                S.op("dve", lambda e_, k_=k_, gsz=gsz: e_.tensor_scalar(lt[k_][:, :gsz], lt[k_][:, :gsz], -7.0, 1.0, ALU.max, ALU.add),
                                     r=[lt[k_].name], w=[lt[k_].name])
                                S.op("dve", lambda e_, k_=k_, gsz=gsz: e_.tensor_tensor(gt[k_][:, :gsz], gt[k_][:, :gsz], st[k_][:, :gsz], ALU.mult),
                                     r=[gt[k_].name, st[k_].name], w=[gt[k_].name])
                                S.op("dve", lambda e_, k_=k_, gsz=gsz, c0=c0, jf=jf, at=at: e_.tensor_tensor(at[:, jf, c0:c0 + gsz], gt[k_][:, :gsz], lt[k_][:, :gsz], ALU.mult),
                                     r=[gt[k_].name, lt[k_].name], w=[(at.name, jf, c0)])
                                c0 += gsz

                    def down(e):
                        at = AT[e % 2]
                        atk = []
                        for jf in range(6):
                            c0 = 0
                            for gsz in groups:
                                atk.append((at.name, jf, c0)); c0 += gsz
                        b_ = bd[e % 2]
                        S.dma("pool", b_[:], bdn_d[e:e + 1, :], w=[b_.name])
                        wv = wdn_d[e].rearrange("(jf p) n -> p jf n", p=128)
                        for dg in range(4):
                            s_ = cnt["d"] % 2; cnt["d"] += 1
                            S.dma("pool", wd[s_][:], wv[:, :, dg * 512:(dg + 1) * 512], w=[wd[s_].name])
                            for tl in range(ntl):
                                p_ = pd[cnt["pd"] % 3]; cnt["pd"] += 1
                                for jf in range(6):
                                    S.op("pe", lambda e_, p_=p_, jf=jf, tl=tl, s_=s_, at=at: e_.matmul(p_[:], at[:, jf, tl * 128:(tl + 1) * 128], wd[s_][:, jf, :], start=(jf == 0), stop=False),
                                         r=atk + [wd[s_].name] if jf == 0 else [wd[s_].name], w=[p_.name] if jf == 0 else [])
                                S.op("pe", lambda e_, p_=p_, dg=dg, b_=b_: e_.matmul(p_[:], ones[0:1, :], b_[0:1, dg * 512:(dg + 1) * 512], start=False, stop=True),
                                     r=atk + ["ones", b_.name], w=[p_.name])
                                gsc = gates[:, t0 + tl, e:e + 1]
                                ysl = Y[:, tl, dg * 512:(dg + 1) * 512]
                                if e == 0:
                                    S.op("dve", lambda e_, p_=p_, gsc=gsc, ysl=ysl: e_.tensor_scalar(ysl, p_[:], gsc, None, ALU.mult),
                                         r=[p_.name, ("gates", t0 + tl)], w=[("Y", tl, dg)])
                                else:
                                    S.op("dve", lambda e_, p_=p_, gsc=gsc, ysl=ysl: e_.scalar_tensor_tensor(ysl, p_[:], gsc, ysl, ALU.mult, ALU.add),
                                         r=[p_.name, ("gates", t0 + tl), ("Y", tl, dg)], w=[("Y", tl, dg)])

                    for e in range(NEXP):
                        up(e)
                        if e > 0:
                            down(e - 1)
                    down(NEXP - 1)
                    S.barrier()
                with ExitStack() as p3:
                    g2B = S.sb("g2B", [128, D], F32, p3)
                    l2g = S.sb("l2g", [128, D], F32, p3)
                    l2b = S.sb("l2b", [128, D], F32, p3)
                    S.dma("sp", l2g[:], ln_d[2:3, :].partition_broadcast(128), w=["l2g"])
                    S.dma("sp", l2b[:], ln_d[3:4, :].partition_broadcast(128), w=["l2b"])
                    x1t = [S.sb(f"x1t{i}", [128, D], F32, p3) for i in range(2)]
                    ot = [S.sb(f"ot{i}", [128, D], F32, p3) for i in range(2)]
                    stats = S.sb("stats3", [128, 4, 6], F32, p3)
                    mv = S.sb("mv3", [128, 2], F32, p3)
                    rstd = S.sb("rstd3", [128, 1], F32, p3)
                    cur = None
                    for tl in range(ntl):
                        i = t0 + tl
                        r_ = 1 if (has_ctx_tile and i == NTT - 1) else 0
                        if cur != r_:
                            S.dma("sp", g2B[:], modscr[r_:r_ + 1, MOD_G2 * D:(MOD_G2 + 1) * D].partition_broadcast(128), r=["modscr"], w=["g2B"])
                            cur = r_
                        xb = x1t[tl % 2]; ob = ot[tl % 2]
                        rows = slice(i * 128, (i + 1) * 128)
                        S.dma("sp", xb[:], x1scr[rows, :], r=[("x1scr", i)], w=[xb.name])
                        yk = [("Y", tl, dg) for dg in range(4)]
                        S.op("pool", lambda e_, tl=tl: e_.tensor_tensor(Y[:, tl, :], Y[:, tl, :], g2B[:], ALU.mult), r=yk + ["g2B"], w=yk)
                        S.op("dve", lambda e_, tl=tl, xb=xb: e_.scalar_tensor_tensor(xb[:], xb[:], ALPHA, Y[:, tl, :], ALU.mult, ALU.add), r=yk + [xb.name], w=[xb.name])
                        _layer_norm_tile(S, xb, [xb.name], ob, [ob.name], l2g, l2b, (stats, mv, rstd))
                        S.dma("sp", out_d[rows, :], ob[:], r=[ob.name], w=[("out", i)])
                    S.barrier()
            t0 += ntl
        for eng in S.ENG:
            S.wait_all(eng)
        print("tail built: ins", S.n_ins, "waits", S.n_wait)
    return nc


HD = 128
ATTN_IN = 4608
QK_EPS = 1e-6


def build_attn_in():
    NT = 2176
    NTT = 17
    nc = bass.Bass("TRN2", target_bir_lowering=False)
    x_d = _dram_in(nc, "x", [NT, D])
    condT_d = _dram_in(nc, "condT", [128, KT, 2])
    ada_w_d = _dram_in(nc, "ada_w", [D, 6 * D])
    ada_b_d = _dram_in(nc, "ada_b", [1, 6 * D])
    win_d = _dram_in(nc, "w_in", [D, ATTN_IN])
    nw_d = _dram_in(nc, "normw", [2, HD])
    cos_d = _dram_in(nc, "cosT", [128, 16, 64])
    sin_d = _dram_in(nc, "sinT", [128, 16, 64])
    ident_d = _dram_in(nc, "ident", [128, 128])
    QT_d = nc.dram_tensor("QT", [16, 128, NT], BF16, kind="ExternalOutput").ap()
    KT_d = nc.dram_tensor("KT", [10, 128, NT], BF16, kind="ExternalOutput").ap()
    V_d = nc.dram_tensor("V", [NT, 1280], BF16, kind="ExternalOutput").ap()
    modscr = nc.dram_tensor("modscr", [2, 2 * D], F32, kind="Internal").ap()
    with ExitStack() as es:
        S = Sched(nc, es)
        ident = S.sb("ident", [128, 128], F32)
        S.dma("sp", ident[:], ident_d, w=["ident"])
        _compute_mod(S, es, condT_d, ada_w_d, ada_b_d, modscr, 0, 2 * D, [(D, 2 * D)])
        hT = S.sb("hTall", [128, KT, NT], BF16)
        with ExitStack() as pa:
            scB = S.sb("scB", [128, D], F32, pa)
            shB = S.sb("shB", [128, D], F32, pa)
            xt = [S.sb(f"xt{i}", [128, D], F32, pa) for i in range(2)]
            pt = [S.ps(f"pt{i}", [128, 512], F32, pa) for i in range(2)]
            tc = 0
            for i in range(NTT):
                if i == 0 or i == NTT - 1:
                    r_ = 0 if i == 0 else 1
                    S.dma("sp", shB[:], modscr[r_:r_ + 1, 0:D].partition_broadcast(128), w=["shB"])
                    S.dma("sp", scB[:], modscr[r_:r_ + 1, D:2 * D].partition_broadcast(128), w=["scB"])
                xb = xt[i % 2]
                S.dma("sp", xb[:], x_d[i * 128:(i + 1) * 128, :], w=[xb.name])
                S.op("dve", lambda e, xb=xb: e.tensor_tensor(xb[:], xb[:], scB[:], ALU.mult), r=[xb.name, "scB"], w=[xb.name])
                S.op("pool", lambda e, xb=xb: e.tensor_tensor(xb[:], xb[:], shB[:], ALU.add), r=[xb.name, "shB"], w=[xb.name])
                for q4 in range(4):
                    p_ = pt[tc % 2]; tc += 1
                    for j in range(4):
                        kt = q4 * 4 + j
                        S.op("pe", lambda e, p_=p_, j=j, kt=kt, xb=xb: e.transpose(p_[:, j * 128:(j + 1) * 128], xb[:, kt * 128:(kt + 1) * 128], ident[:]),
                             r=[xb.name, "ident"], w=[p_.name])
                    S.op("act", lambda e, p_=p_, q4=q4, i=i: e.activation(out=hT[:, q4 * 4:(q4 + 1) * 4, i * 128:(i + 1) * 128], in_=p_[:].rearrange("p (a b) -> p a b", a=4), func=AF.Copy),
                         r=[p_.name], w=[("hT", i)])
            S.barrier()
        with ExitStack() as pb:
            cosT = S.sb("cosT", [128, 16, 64], F32, pb)
            sinT = S.sb("sinT", [128, 16, 64], F32, pb)
            S.dma("sp", cosT[:], cos_d, w=["cos"]); S.dma("sp", sinT[:], sin_d, w=["sin"])
            qnw = S.sb("qnw", [128, HD], F32, pb); knw = S.sb("knw", [128, HD], F32, pb)
            S.dma("sp", qnw[:], nw_d[0:1, :].partition_broadcast(128), w=["qnw"])
            S.dma("sp", knw[:], nw_d[1:2, :].partition_broadcast(128), w=["knw"])
            wb = [S.sb(f"winb{i}", [128, KT, 512], BF16, pb) for i in range(2)]
            pq = [S.ps(f"pq{i}", [128, 512], F32, pb) for i in range(2)]
            pt = [S.ps(f"ptb{i}", [128, 512], F32, pb) for i in range(2)]
            xq = [S.sb(f"xq{i}", [128, 512], F32, pb) for i in range(2)]
            xr = [S.sb(f"xr{i}", [128, 512], F32, pb) for i in range(2)]
            t1 = S.sb("t1", [128, 256], F32, pb); t2 = S.sb("t2", [128, 256], F32, pb)
            sq = S.sb("sq", [128, 512], F32, pb)
            ss = S.sb("ss", [128, 4], F32, pb)
            ob = [S.sb(f"ob{i}", [128, 4, 128], BF16, pb) for i in range(2)]
            vb = [S.sb(f"vbf{i}", [128, 512], BF16, pb) for i in range(2)]
            wv = win_d.rearrange("(kt p) n -> p kt n", p=128)
            u = 0
            for cg in range(9):
                w_ = wb[cg % 2]
                S.dma("pool", w_[:], wv[:, :, cg * 512:(cg + 1) * 512], w=[w_.name])
                kind = ["q", "q", "k", "k", "v", "v", "qn", "qn", "kv"][cg]
                for i in range(NTT):
                    k_ = u % 2; u += 1
                    p_ = pq[k_]
                    for kt in range(KT):
                        S.op("pe", lambda e, p_=p_, kt=kt, i=i, w_=w_: e.matmul(p_[:], hT[:, kt, i * 128:(i + 1) * 128], w_[:, kt, :], start=(kt == 0), stop=(kt == KT - 1)),
                             r=[("hT", i), w_.name], w=[p_.name] if kt in (0, KT - 1) else [])
                    x_ = xq[k_]
                    S.op("act", lambda e, x_=x_, p_=p_: e.activation(out=x_[:], in_=p_[:], func=AF.Copy), r=[p_.name], w=[x_.name])
                    rows = slice(i * 128, (i + 1) * 128)
                    nrope = 4 if kind in ("q", "k", "qn") else (2 if kind == "kv" else 0)
                    nnorm = 4 if kind == "qn" else (2 if kind == "kv" else 0)
                    if nnorm:
                        nwt = qnw if kind == "qn" else knw
                        xv = x_[:, :nnorm * 128].rearrange("p (h f) -> p h f", h=nnorm)
                        sv = sq[:, :nnorm * 128].rearrange("p (h f) -> p h f", h=nnorm)
                        S.op("dve", lambda e, xv=xv, sv=sv: e.tensor_tensor(sv, xv, xv, ALU.mult), r=[x_.name], w=["sq"])
                        S.op("dve", lambda e, sv=sv, nnorm=nnorm: e.reduce_sum(out=ss[:, :nnorm], in_=sv, axis=AX.X), r=["sq"], w=["ss"])
                        S.op("dve", lambda e, nnorm=nnorm: e.tensor_scalar(ss[:, :nnorm], ss[:, :nnorm], 1.0 / HD, QK_EPS, ALU.mult, ALU.add), r=["ss"], w=["ss"])
                        S.op("act", lambda e, nnorm=nnorm: e.activation(out=ss[:, :nnorm], in_=ss[:, :nnorm], func=AF.Sqrt), r=["ss"], w=["ss"])
                        S.op("dve", lambda e, nnorm=nnorm: e.reciprocal(ss[:, :nnorm], ss[:, :nnorm]), r=["ss"], w=["ss"])
                        S.op("dve", lambda e, xv=xv, nnorm=nnorm: e.tensor_tensor(xv, xv, ss[:, :nnorm].unsqueeze(2).to_broadcast([128, nnorm, HD]), ALU.mult), r=[x_.name, "ss"], w=[x_.name])
                        S.op("pool", lambda e, xv=xv, nnorm=nnorm, nwt=nwt: e.tensor_tensor(xv, xv, nwt[:].unsqueeze(1).to_broadcast([128, nnorm, HD]), ALU.mult), r=[x_.name, nwt.name, "qnw", "knw"], w=[x_.name])
                    src = x_
                    if nrope and i < 16:
                        xo_ = xr[k_]
                        x4 = x_[:, :nrope * 128].rearrange("p (h t f) -> p h t f", h=nrope, t=2)
                        o4 = xo_[:, :nrope * 128].rearrange("p (h t f) -> p h t f", h=nrope, t=2)
                        cb = cosT[:, i, :].unsqueeze(1).to_broadcast([128, nrope, 64])
                        sb_ = sinT[:, i, :].unsqueeze(1).to_broadcast([128, nrope, 64])
                        t1v = t1[:, :nrope * 64].rearrange("p (h f) -> p h f", h=nrope)
                        t2v = t2[:, :nrope * 64].rearrange("p (h f) -> p h f", h=nrope)
                        S.op("dve", lambda e, x4=x4, cb=cb, t1v=t1v: e.tensor_tensor(t1v, x4[:, :, 0, :], cb, ALU.mult), r=[x_.name, "cos"], w=["t1"])
                        S.op("pool", lambda e, x4=x4, sb_=sb_, t2v=t2v: e.tensor_tensor(t2v, x4[:, :, 1, :], sb_, ALU.mult), r=[x_.name, "sin"], w=["t2"])
                        S.op("dve", lambda e, o4=o4, t1v=t1v, t2v=t2v: e.tensor_tensor(o4[:, :, 0, :], t1v, t2v, ALU.subtract), r=["t1", "t2"], w=[xo_.name])
                        S.op("dve", lambda e, x4=x4, cb=cb, t1v=t1v: e.tensor_tensor(t1v, x4[:, :, 1, :], cb, ALU.mult), r=[x_.name, "cos"], w=["t1"])
                        S.op("pool", lambda e, x4=x4, sb_=sb_, t2v=t2v: e.tensor_tensor(t2v, x4[:, :, 0, :], sb_, ALU.mult), r=[x_.name, "sin"], w=["t2"])
                        S.op("dve", lambda e, o4=o4, t1v=t1v, t2v=t2v: e.tensor_tensor(o4[:, :, 1, :], t1v, t2v, ALU.add), r=["t1", "t2"], w=[xo_.name])
                        if nrope < 4:
                            S.op("pool", lambda e, xo_=xo_, x_=x_: e.tensor_copy(xo_[:, 256:512], x_[:, 256:512]), r=[x_.name], w=[xo_.name])
                        src = xo_
                    nT = 4 if kind in ("q", "k", "qn") else (2 if kind == "kv" else 0)
                    if nT:
                        pp = pt[k_]
                        for j in range(nT):
                            S.op("pe", lambda e, pp=pp, j=j, src=src: e.transpose(pp[:, j * 128:(j + 1) * 128], src[:, j * 128:(j + 1) * 128], ident[:]),
                                 r=[src.name, "ident"], w=[pp.name])
                        o_ = ob[k_]
                        S.op("act", lambda e, pp=pp, o_=o_, nT=nT: e.activation(out=o_[:, :nT, :], in_=pp[:, :nT * 128].rearrange("p (a b) -> p a b", a=nT), func=AF.Copy),
                             r=[pp.name], w=[o_.name])
                        if kind == "q":
                            dst = QT_d[cg * 4:(cg + 1) * 4, :, rows]
                        elif kind == "qn":
                            dst = QT_d[8 + (cg - 6) * 4: 8 + (cg - 5) * 4, :, rows]
                        elif kind == "k":
                            dst = KT_d[(cg - 2) * 4:(cg - 1) * 4, :, rows]
                        else:
                            dst = KT_d[8:10, :, rows]
                        S.dma("sp", dst.rearrange("h p t -> p h t"), o_[:, :nT, :], r=[o_.name], w=[("QKo", cg, i)])
                    if kind in ("v", "kv"):
                        v_ = vb[k_]
                        if kind == "v":
                            S.op("dve", lambda e, v_=v_, src=src: e.tensor_copy(v_[:], src[:]), r=[src.name], w=[v_.name])
                            S.dma("sp", V_d[rows, (cg - 4) * 512:(cg - 3) * 512], v_[:], r=[v_.name], w=[("Vo", cg, i)])
                        else:
                            S.op("dve", lambda e, v_=v_, src=src: e.tensor_copy(v_[:, 256:512], src[:, 256:512]), r=[src.name], w=[v_.name])
                            S.dma("sp", V_d[rows, 1024:1280], v_[:, 256:512], r=[v_.name], w=[("Vo", cg, i)])
            S.barrier()
        for eng in S.ENG:
            S.wait_all(eng)
        print("attn_in built: ins", S.n_ins, "waits", S.n_wait)
    return nc


SUBLN_EPS = 1e-5


def build_attn(need_ctx, lam_init):
    NQ = 2176 if need_ctx else 2048
    NK = 4352
    NKT = NK // 128
    nc = bass.Bass("TRN2", target_bir_lowering=False)
    QT_d = _dram_in(nc, "QT", [16, 128, 2176], BF16)
    KT_d = _dram_in(nc, "KT", [10, 128, NK], BF16)
    V_d = _dram_in(nc, "V", [NK, 1280], BF16)
    lamv_d = _dram_in(nc, "lamv", [4, HD])
    subw_d = _dram_in(nc, "subw", [1, 256])
    m_d = nc.dram_tensor("m", [NQ, D], F32, kind="ExternalOutput").ap()
    scale = HD ** -0.5
    qblocks = [(q0, 512, NKT) for q0 in range(0, 2048, 512)] + ([(2048, 128, 2)] if need_ctx else [])
    with ExitStack() as es:
        S = Sched(nc, es)
        lam4 = [S.sb(f"lam{i}", [128, HD], F32) for i in range(4)]
        for i in range(4):
            S.dma("sp", lam4[i][:], lamv_d[i:i + 1, :].partition_broadcast(128), w=[f"lam{i}"])
        e12 = S.sb("e12", [128, 2], F32)
        nlam = S.sb("nlam", [128, 1], F32)
        for j in range(2):
            S.op("dve", lambda e, j=j: e.tensor_tensor(lam4[2 * j][:], lam4[2 * j][:], lam4[2 * j + 1][:], ALU.mult), r=[f"lam{2*j}", f"lam{2*j+1}"], w=[f"lam{2*j}"])
            S.op("dve", lambda e, j=j: e.reduce_sum(out=e12[:, j:j + 1], in_=lam4[2 * j][:], axis=AX.X), r=[f"lam{2*j}"], w=[("e12", j)])
        S.op("act", lambda e: e.activation(out=e12[:], in_=e12[:], func=AF.Exp), r=[("e12", 0), ("e12", 1)], w=[("e12", 0), ("e12", 1)])
        S.op("dve", lambda e: e.tensor_tensor(nlam[:], e12[:, 1:2], e12[:, 0:1], ALU.subtract), r=[("e12", 0), ("e12", 1)], w=["nlam"])
        S.op("dve", lambda e: e.tensor_scalar(nlam[:], nlam[:], -lam_init, None, ALU.add), r=["nlam"], w=["nlam"])
        wsub = S.sb("wsub", [128, 256], F32)
        S.dma("sp", wsub[:], subw_d.partition_broadcast(128), w=["wsub"])
        S.op("dve", lambda e: e.tensor_scalar(wsub[:], wsub[:], 1.0 - lam_init, None, ALU.mult), r=["wsub"], w=["wsub"])
        Kb = [S.sb(f"Kb{i}", [128, 2, NK], BF16) for i in range(2)]
        Qb = [S.sb(f"Qb{i}", [128, 4, 2176], BF16) for i in range(2)]
        Vb = [S.sb(f"Vb{i}", [128, NKT, 257], BF16) for i in range(2)]
        pS = [S.ps(f"pS{i}", [128, 512], F32) for i in range(2)]
        acc = [S.ps(f"acc{i}", [128, 512], F32) for i in range(4)]
        pT = [S.sb(f"pT{i}", [128, 512], BF16) for i in range(3)]
        o0 = S.sb("o0", [128, 4, 256], F32)
        dd = S.sb("dd", [128, 256], F32)
        sq = S.sb("sq", [128, 256], F32)
        ssum = S.sb("ssum", [128, 1], F32)
        rec = S.sb("rec", [128, 1], F32)
        stg = [S.sb(f"stg{i}", [128, 4, 256], F32) for i in range(2)]
        Vv = V_d.rearrange("(kt p) c -> p kt c", p=128)
        cnt = {"s": 0, "t": 0, "g": 0}

        def attend(unit_q, unit_k, vset, dv, q0, qn, nkt, qset, kset):
            nqt = qn // 128
            for kt in range(nkt):
                ps_ = pS[cnt["s"] % 2]; cnt["s"] += 1
                S.op("pe", lambda e, ps_=ps_, kt=kt: e.matmul(ps_[:, :qn], Kb[kset][:, unit_k, kt * 128:(kt + 1) * 128], Qb[qset][:, unit_q, q0:q0 + qn], start=True, stop=True),
                     r=[Kb[kset].name, Qb[qset].name], w=[ps_.name])
                pt_ = pT[cnt["t"] % 3]; cnt["t"] += 1
                S.op("act", lambda e, ps_=ps_, pt_=pt_: e.activation(out=pt_[:, :qn], in_=ps_[:, :qn], func=AF.Exp, scale=scale), r=[ps_.name], w=[pt_.name])
                for qt in range(nqt):
                    S.op("pe", lambda e, qt=qt, pt_=pt_, kt=kt: e.matmul(acc[qt][:, :dv + 1], pt_[:, qt * 128:(qt + 1) * 128], Vb[vset][:, kt, :dv + 1], start=(kt == 0), stop=(kt == nkt - 1)),
                         r=[pt_.name, Vb[vset].name], w=[acc[qt].name] if kt in (0, nkt - 1) else [])

        ph = 0
        for h in range(4):
            s_ = ph % 2; ph += 1
            for m_ in range(2):
                S.dma("sp", Kb[s_][:, m_, :], KT_d[2 * h + m_], w=[Kb[s_].name])
                S.dma("sp", Qb[s_][:, m_, :], QT_d[2 * h + m_], w=[Qb[s_].name])
            S.op("pool", lambda e, s_=s_: e.memset(Vb[s_][:, :, 256:257], 1.0), w=[Vb[s_].name])
            S.dma("sp", Vb[s_][:, :, 0:256], Vv[:, :, h * 256:(h + 1) * 256], w=[Vb[s_].name])
            for (q0, qn, nkt) in qblocks:
                nqt = qn // 128
                st_ = stg[cnt["g"] % 2]; cnt["g"] += 1
                for m_ in range(2):
                    attend(m_, m_, s_, 256, q0, qn, nkt, s_, s_)
                    for qt in range(nqt):
                        a_ = acc[qt]
                        S.op("dve", lambda e, a_=a_: e.reciprocal(rec[:], a_[:, 256:257]), r=[a_.name], w=["rec"])
                        if m_ == 0:
                            S.op("dve", lambda e, a_=a_, qt=qt: e.tensor_scalar(o0[:, qt, :], a_[:, 0:256], rec[:], None, ALU.mult), r=[a_.name, "rec"], w=[("o0", qt)])
                        else:
                            S.op("dve", lambda e, a_=a_: e.tensor_scalar(dd[:], a_[:, 0:256], rec[:], nlam[:], ALU.mult, ALU.mult), r=[a_.name, "rec", "nlam"], w=["dd"])
                            S.op("dve", lambda e, qt=qt: e.tensor_tensor(dd[:], dd[:], o0[:, qt, :], ALU.add), r=["dd", ("o0", qt)], w=["dd"])
                            S.op("pool", lambda e: e.tensor_tensor(sq[:], dd[:], dd[:], ALU.mult), r=["dd"], w=["sq"])
                            S.op("dve", lambda e: e.reduce_sum(out=ssum[:], in_=sq[:], axis=AX.X), r=["sq"], w=["ssum"])
                            S.op("dve", lambda e: e.tensor_scalar(ssum[:], ssum[:], 1.0 / 256, SUBLN_EPS, ALU.mult, ALU.add), r=["ssum"], w=["ssum"])
                            S.op("act", lambda e: e.activation(out=ssum[:], in_=ssum[:], func=AF.Ln), r=["ssum"], w=["ssum"])
                            S.op("act", lambda e: e.activation(out=ssum[:], in_=ssum[:], func=AF.Exp, scale=-0.5), r=["ssum"], w=["ssum"])
                            S.op("dve", lambda e, st_=st_, qt=qt: e.scalar_tensor_tensor(st_[:, qt, :], dd[:], ssum[:], wsub[:], ALU.mult, ALU.mult), r=["dd", "ssum", "wsub"], w=[st_.name])
                S.dma("sp", m_d[q0:q0 + qn, h * 256:(h + 1) * 256].rearrange("(qt p) c -> p qt c", p=128), st_[:, :nqt, :], r=[st_.name], w=[("m", h, q0)])
        for g2 in range(2):
            s_ = ph % 2; ph += 1
            S.dma("sp", Kb[s_][:, 0, :], KT_d[8 + g2], w=[Kb[s_].name])
            for j in range(4):
                S.dma("sp", Qb[s_][:, j, :], QT_d[8 + 4 * g2 + j], w=[Qb[s_].name])
            S.op("pool", lambda e, s_=s_: e.memset(Vb[s_][:, :, 128:129], 1.0), w=[Vb[s_].name])
            S.dma("sp", Vb[s_][:, :, 0:128], Vv[:, :, 1024 + g2 * 128:1024 + (g2 + 1) * 128], w=[Vb[s_].name])
            for (q0, qn, nkt) in qblocks:
                nqt = qn // 128
                for j2 in range(2):
                    st_ = stg[cnt["g"] % 2]; cnt["g"] += 1
                    for jj in range(2):
                        j = j2 * 2 + jj
                        attend(j, 0, s_, 128, q0, qn, nkt, s_, s_)
                        for qt in range(nqt):
                            a_ = acc[qt]
                            S.op("dve", lambda e, a_=a_: e.reciprocal(rec[:], a_[:, 128:129]), r=[a_.name], w=["rec"])
                            S.op("dve", lambda e, a_=a_, qt=qt, jj=jj, st_=st_: e.tensor_scalar(st_[:, qt, jj * 128:(jj + 1) * 128], a_[:, 0:128], rec[:], None, ALU.mult), r=[a_.name, "rec"], w=[st_.name])
                    c0 = 1024 + (4 * g2 + j2 * 2) * 128
                    S.dma("sp", m_d[q0:q0 + qn, c0:c0 + 256].rearrange("(qt p) c -> p qt c", p=128), st_[:, :nqt, :], r=[st_.name], w=[("m", 4 + g2, q0, j2)])
        S.barrier()
        for eng in S.ENG:
            S.wait_all(eng)
        print("attn built: ins", S.n_ins, "waits", S.n_wait)
    return nc


TWO_PI = 2.0 * math.pi


def build_hyena(has_ctx):
    NL = 4 * 4096
    NC_ = 4 * 256 if has_ctx else 0
    NTOK = NL + NC_
    nc = bass.Bass("TRN2", target_bir_lowering=False)
    x_d = _dram_in(nc, "xr", [NTOK, D])
    condT_d = _dram_in(nc, "condT", [128, KT, 5])
    ada_w_d = _dram_in(nc, "ada_w", [D, 6 * D])
    ada_b_d = _dram_in(nc, "ada_b", [1, 6 * D])
    win_d = _dram_in(nc, "w_in", [D, 768])
    vec_d = _dram_in(nc, "vecs", [5, 768])
    fw1_d = _dram_in(nc, "fw1", [33, 64])
    fw23_d = _dram_in(nc, "fw23", [2, 64, 64])
    fvec_d = _dram_in(nc, "fvec", [64, 4])
    fw4_d = _dram_in(nc, "fw4", [64, 512])
    lb_d = _dram_in(nc, "lb", [128, 2])
    zemb_d = _dram_in(nc, "zemb", [33, 8192])
    dec_d = _dram_in(nc, "decay", [128, 2, 8192])
    if has_ctx:
        zembc_d = _dram_in(nc, "zembc", [33, 512])
        decc_d = _dram_in(nc, "decayc", [128, 2, 512])
    ident_d = _dram_in(nc, "ident", [128, 128])
    shm_d = _dram_in(nc, "shm", [4, 128, 128])
    yc_d = nc.dram_tensor("yc", [NTOK, 256], F32, kind="ExternalOutput").ap()
    x0_d = nc.dram_tensor("x0o", [NTOK, 256], F32, kind="ExternalOutput").ap()
    modscr = nc.dram_tensor("modscr", [5, 2 * D], F32, kind="Internal").ap()
    Kd = nc.dram_tensor("Kd", [256, 8192], BF16, kind="Internal").ap()
    Kdc = nc.dram_tensor("Kdc", [256, 512], BF16, kind="Internal").ap()
    with ExitStack() as es:
        S = Sched(nc, es)
        ident = S.sb("ident", [128, 128], F32)
        S.dma("sp", ident[:], ident_d, w=["ident"])
        _compute_mod(S, es, condT_d, ada_w_d, ada_b_d, modscr, 0, 2 * D, [(D, 2 * D)], nrows=5)
        with ExitStack() as pf:
            fw1 = S.sb("fw1", [33, 64], F32, pf); S.dma("sp", fw1[:], fw1_d, w=["fw1"])
            fw2 = S.sb("fw2", [64, 64], F32, pf); S.dma("sp", fw2[:], fw23_d[0], w=["fw2"])
            fw3 = S.sb("fw3", [64, 64], F32, pf); S.dma("sp", fw3[:], fw23_d[1], w=["fw3"])
            fw4 = S.sb("fw4", [64, 512], F32, pf); S.dma("sp", fw4[:], fw4_d, w=["fw4"])
            fvec = S.sb("fvec", [64, 4], F32, pf); S.dma("sp", fvec[:], fvec_d, w=["fvec"])
            lb = S.sb("lb", [128, 2], F32, pf); S.dma("sp", lb[:], lb_d, w=["lb"])
            fs = S.sb("fs", [64, 4], F32, pf)
            S.op("dve", lambda e: e.tensor_scalar(fs[:, 3:4], fvec[:, 3:4], 1.0 / TWO_PI, None, ALU.mult), r=["fvec"], w=["fs3"])
            S.op("dve", lambda e: e.tensor_scalar(fs[:, 0:3], fvec[:, 0:3], fs[:, 3:4], None, ALU.mult), r=["fvec", "fs3"], w=["fs"])
            zemb = S.sb("zemb", [33, 8192], F32, pf)
            S.dma("sp", zemb[:], zemb_d, w=["zemb"])
            dec = S.sb("dec", [128, 2, 8192], F32, pf)
            S.dma("sp", dec[:], dec_d, w=["dec"])
            if has_ctx:
                zembc = S.sb("zembc", [33, 512], F32, pf); S.dma("sp", zembc[:], zembc_d, w=["zembc"])
                decc = S.sb("decc", [128, 2, 512], F32, pf); S.dma("sp", decc[:], decc_d, w=["decc"])
            pa = [S.ps(f"pa{i}", [64, 512], F32, pf) for i in range(2)]
            p4 = [S.ps(f"p4{i}", [128, 512], F32, pf) for i in range(2)]
            vt = S.sb("vt", [64, 512], F32, pf)
            vi = S.sb("vi", [64, 512], mybir.dt.int32, pf)
            vf = S.sb("vf", [64, 512], F32, pf)
            at = [S.sb(f"fa{i}", [64, 512], F32, pf) for i in range(3)]
            hk = S.sb("hk", [128, 512], F32, pf)
            hkb = [S.sb(f"hkb{i}", [128, 512], BF16, pf) for i in range(2)]
            cnt = {"a": 0, "k": 0}

            def sin_layer(ps_, li, out_):
                S.op("dve", lambda e: e.tensor_scalar(vt[:], ps_[:], fs[:, 3:4], fs[:, li:li + 1], ALU.mult, ALU.add), r=[ps_.name, "fs", "fs3"], w=["vt"])
                S.op("dve", lambda e: e.tensor_copy(vi[:], vt[:]), r=["vt"], w=["vi"])
                S.op("dve", lambda e: e.tensor_copy(vf[:], vi[:]), r=["vi"], w=["vf"])
                S.op("dve", lambda e: e.tensor_tensor(vt[:], vt[:], vf[:], ALU.subtract), r=["vt", "vf"], w=["vt"])
                S.op("act", lambda e: e.activation(out=out_[:], in_=vt[:], func=AF.Sin, scale=TWO_PI), r=["vt"], w=[out_.name])

            def gen(zsrc, zkey, g, dsrc, dkey, dcol0, plan, Kdst, kcol0):
                pA = pa[cnt["a"] % 2]; cnt["a"] += 1
                S.op("pe", lambda e: e.matmul(pA[:], fw1[:], zsrc[:, g * 512:(g + 1) * 512], start=True, stop=True), r=["fw1", zkey], w=[pA.name])
                sin_layer(pA, 0, at[0])
                pB = pa[cnt["a"] % 2]; cnt["a"] += 1
                S.op("pe", lambda e: e.matmul(pB[:], fw2[:], at[0][:], start=True, stop=True), r=["fw2", at[0].name], w=[pB.name])
                sin_layer(pB, 1, at[1])
                pC = pa[cnt["a"] % 2]; cnt["a"] += 1
                S.op("pe", lambda e: e.matmul(pC[:], fw3[:], at[1][:], start=True, stop=True), r=["fw3", at[1].name], w=[pC.name])
                sin_layer(pC, 2, at[2])
                for j in range(2):
                    p_ = p4[cnt["k"] % 2]
                    hb = hkb[cnt["k"] % 2]; cnt["k"] += 1
                    for (lo, hi, dr) in plan:
                        c0 = (0 if dr == "fwd" else 256) + j * 128
                        S.op("pe", lambda e, lo=lo, hi=hi, c0=c0: e.matmul(p_[:, lo:hi], fw4[:, c0:c0 + 128], at[2][:, lo:hi], start=True, stop=True), r=["fw4", at[2].name], w=[p_.name])
                    S.op("dve", lambda e, j=j: e.tensor_tensor(hk[:], p_[:], dsrc[:, j, dcol0:dcol0 + 512], ALU.mult), r=[p_.name, dkey], w=["hk"])
                    for (lo, hi, dr) in plan:
                        if dr == "fwd0":
                            pass
                    yield_j = j
                    for (lo, hi, dr, zero_col) in [(a, b_, c_, None) for (a, b_, c_) in plan]:
                        pass
                    if ("lbcol", g) in gen.lbcols:
                        col = gen.lbcols[("lbcol", g)]
                        S.op("dve", lambda e, j=j, col=col: e.tensor_tensor(hk[:, col:col + 1], hk[:, col:col + 1], lb[:, j:j + 1], ALU.add), r=["hk", "lb"], w=["hk"])
                    S.op("dve", lambda e, hb=hb: e.tensor_copy(hb[:], hk[:]), r=["hk"], w=[hb.name])
                    S.dma("sp", Kdst[j * 128:(j + 1) * 128, kcol0:kcol0 + 512], hb[:], r=[hb.name], w=[("K", id(Kdst), j, kcol0)])

            for g in range(16):
                gen.lbcols = {("lbcol", 8): 0}
                gen(zemb, "zemb", g, dec, "dec", g * 512, [(0, 512, "bwd" if g < 8 else "fwd")], Kd, g * 512)
            if has_ctx:
                gen.lbcols = {("lbcol", 0): 256}
                gen(zembc, "zembc", 0, decc, "decc", 0, [(0, 256, "bwd"), (256, 512, "fwd")], Kdc, 0)
            S.barrier()

        ZT = S.sb("ZT", [128, 256, 4, 32], BF16)
        ZTc = S.sb("ZTc", [128, 256, 4, 2], BF16) if has_ctx else None
        with ExitStack() as p1:
            W = S.sb("W", [128, KT, 768], BF16, p1)
            S.dma("pool", W[:], win_d.rearrange("(kt p) n -> p kt n", p=128), w=["W"])
            vB = [S.sb(f"vB{i}", [128, 768], F32, p1) for i in range(5)]
            for i in range(5):
                S.dma("sp", vB[i][:], vec_d[i:i + 1, :].partition_broadcast(128), w=[f"vB{i}"])
            shm = [S.sb(f"shm{i}", [128, 128], F32, p1) for i in range(4)]
            for i in range(4):
                S.dma("sp", shm[i][:], shm_d[i], w=[f"shm{i}"])
            scB = S.sb("scB", [128, D], F32, p1)
            shB = S.sb("shB", [128, D], F32, p1)
            xt = [S.sb(f"xt{i}", [128, D], F32, p1) for i in range(2)]
            hT = [S.sb(f"hT{i}", [128, KT, 128], BF16, p1) for i in range(2)]
            pr = [S.sb(f"pr{i}", [128, 768], F32, p1) for i in range(4)]
            ut = [S.sb(f"ut{i}", [128, 768], F32, p1) for i in range(2)]
            tt = [S.sb(f"tt{i}", [128, 384], F32, p1) for i in range(2)]
            pt = [S.ps(f"pt{i}", [128, 512], F32, p1) for i in range(2)]
            pp = [S.ps(f"pp{i}", [128, 384], F32, p1) for i in range(2)]
            psh = [S.ps(f"psh{i}", [128, 384], F32, p1) for i in range(4)]
            cnt = {"t": 0, "x": 0}
            seqs = [(b, "l", b * 4096, 32, b) for b in range(4)]
            if has_ctx:
                seqs += [(b, "c", NL + b * 256, 2, 4) for b in range(4)]
            cur_mod = None

            def stageA(tok0, slot, mrow):
                nonlocal cur_mod
                if cur_mod != mrow:
                    S.dma("sp", shB[:], modscr[mrow:mrow + 1, 0:D].partition_broadcast(128), w=["shB"])
                    S.dma("sp", scB[:], modscr[mrow:mrow + 1, D:2 * D].partition_broadcast(128), w=["scB"])
                    cur_mod = mrow
                k_ = cnt["x"] % 2; cnt["x"] += 1
                xb = xt[k_]; hb = hT[k_]
                S.dma("sp", xb[:], x_d[tok0:tok0 + 128, :], w=[xb.name])
                S.op("dve", lambda e: e.tensor_tensor(xb[:], xb[:], scB[:], ALU.mult), r=[xb.name, "scB"], w=[xb.name])
                S.op("pool", lambda e: e.tensor_tensor(xb[:], xb[:], shB[:], ALU.add), r=[xb.name, "shB"], w=[xb.name])
                for q4 in range(4):
                    p_ = pt[cnt["t"] % 2]; cnt["t"] += 1
                    for j in range(4):
                        kt = q4 * 4 + j
                        S.op("pe", lambda e, p_=p_, j=j, kt=kt: e.transpose(p_[:, j * 128:(j + 1) * 128], xb[:, kt * 128:(kt + 1) * 128], ident[:]),
                             r=[xb.name, "ident"], w=[p_.name])
                    S.op("act", lambda e, p_=p_, q4=q4: e.activation(out=hb[:, q4 * 4:(q4 + 1) * 4, :], in_=p_[:].rearrange("p (a b) -> p a b", a=4), func=AF.Copy),
                         r=[p_.name], w=[(hb.name, q4)])
                pslot = pr[slot]
                for hf in range(2):
                    for kt in range(KT):
                        S.op("pe", lambda e, hf=hf, kt=kt: e.matmul(pp[hf][:], hb[:, kt, :], W[:, kt, hf * 384:(hf + 1) * 384], start=(kt == 0), stop=(kt == KT - 1)),
                             r=[(hb.name, kt // 4), "W"], w=[pp[hf].name] if kt in (0, KT - 1) else [])
                    S.op("dve", lambda e, hf=hf: e.tensor_tensor(pslot[:, hf * 384:(hf + 1) * 384], pp[hf][:], vB[0][:, hf * 384:(hf + 1) * 384], ALU.add),
                         r=[pp[hf].name, "vB0"], w=[(pslot.name, hf)])

            def stageB(slot, slot_prev, slot_next, tok0, b, J, kind):
                p_i = pr[slot]
                u = ut[J % 2]
                pk = [(p_i.name, 0), (p_i.name, 1)]
                for hf in range(2):
                    cs = slice(hf * 384, (hf + 1) * 384)
                    a_ = psh[hf]; n_ = psh[2 + hf]
                    S.op("pe", lambda e, a_=a_, cs=cs: e.matmul(a_[:], shm[0][:], p_i[:, cs], start=True, stop=(slot_prev is None)), r=["shm0"] + pk, w=[a_.name])
                    if slot_prev is not None:
                        pp_ = pr[slot_prev]
                        S.op("pe", lambda e, a_=a_, cs=cs, pp_=pp_: e.matmul(a_[:], shm[1][:], pp_[:, cs], start=False, stop=True), r=["shm1", (pp_.name, 0), (pp_.name, 1)], w=[a_.name])
                    S.op("pe", lambda e, n_=n_, cs=cs: e.matmul(n_[:], shm[2][:], p_i[:, cs], start=True, stop=(slot_next is None)), r=["shm2"] + pk, w=[n_.name])
                    if slot_next is not None:
                        pn_ = pr[slot_next]
                        S.op("pe", lambda e, n_=n_, cs=cs, pn_=pn_: e.matmul(n_[:], shm[3][:], pn_[:, cs], start=False, stop=True), r=["shm3", (pn_.name, 0), (pn_.name, 1)], w=[n_.name])
                    t_ = tt[hf]
                    S.op("pool", lambda e, cs=cs: e.tensor_tensor(u[:, cs], p_i[:, cs], vB[2][:, cs], ALU.mult), r=pk + ["vB2"], w=[(u.name, hf)])
                    S.op("pool", lambda e, cs=cs: e.tensor_tensor(u[:, cs], u[:, cs], vB[4][:, cs], ALU.add), r=[(u.name, hf), "vB4"], w=[(u.name, hf)])
                    S.op("dve", lambda e, cs=cs, a_=a_, t_=t_: e.tensor_tensor(t_[:], a_[:], vB[1][:, cs], ALU.mult), r=[a_.name, "vB1"], w=[t_.name])
                    S.op("pool", lambda e, cs=cs, t_=t_: e.tensor_tensor(u[:, cs], u[:, cs], t_[:], ALU.add), r=[(u.name, hf), t_.name], w=[(u.name, hf)])
                    S.op("dve", lambda e, cs=cs, n_=n_, t_=t_: e.tensor_tensor(t_[:], n_[:], vB[3][:, cs], ALU.mult), r=[n_.name, "vB3"], w=[t_.name])
                    S.op("pool", lambda e, cs=cs, t_=t_: e.tensor_tensor(u[:, cs], u[:, cs], t_[:], ALU.add), r=[(u.name, hf), t_.name], w=[(u.name, hf)])
                zt = ZT if kind == "l" else ZTc
                S.op("dve", lambda e: e.tensor_tensor(zt[:, :, b, J], u[:, 512:768], u[:, 256:512], ALU.mult), r=[(u.name, 0), (u.name, 1)], w=[("ZT", kind, b, J)])
                S.dma("sp", x0_d[tok0:tok0 + 128, :], u[:, 0:256], r=[(u.name, 0)], w=[("x0o", tok0)])

            slot_ctr = 0
            for (b, kind, base, ntile, mrow) in seqs:
                slots = []
                for J in range(ntile):
                    sl = slot_ctr % 4; slot_ctr += 1
                    slots.append(sl)
                    stageA(base + J * 128, sl, mrow)
                    if J >= 1:
                        stageB(slots[J - 1], slots[J - 2] if J >= 2 else None, slots[J], base + (J - 1) * 128, b, J - 1, kind)
                stageB(slots[ntile - 1], slots[ntile - 2], None, base + (ntile - 1) * 128, b, ntile - 1, kind)
            S.barrier()

        with ExitStack() as p2:
            Ab = [S.sb(f"Ab{i}", [128, 8064], BF16, p2) for i in range(2)]
            Yst = S.sb("Yst", [128, 4, 32, 64], F32, p2)
            po = [S.ps(f"po{i}", [128, 4, 4, 32], F32, p2) for i in range(2)]
            dlist = [0] + [d for d in range(-31, 32) if d != 0]
            for c in range(256):
                a_ = Ab[c % 2]
                S.dma("sp", a_[:], bass.AP(Kd.tensor, c * 8192 + 1, [[1, 128], [1, 8064]]), w=[a_.name])
                p_ = po[(c // 4) % 2]
                for di, d in enumerate(dlist):
                    if d >= 0:
                        J0, J1, I0, I1 = 0, 32 - d, d, 32
                    else:
                        J0, J1, I0, I1 = -d, 32, 0, 32 + d
                    S.op("pe", lambda e, d=d, J0=J0, J1=J1, I0=I0, I1=I1, a_=a_, p_=p_, c=c, di=di: e.matmul(
                        p_[:, c % 4, :, I0:I1], a_[:, 128 * (d + 31):128 * (d + 32)], ZT[:, c, :, J0:J1], start=(di == 0), stop=(di == len(dlist) - 1)),
                        r=[a_.name], w=[(p_.name, c % 4)] if di in (0, len(dlist) - 1) else [])
                if c % 4 == 3:
                    cs = (c % 64) - 3
                    S.op("act", lambda e, p_=p_, cs=cs: e.activation(out=Yst[:, :, :, cs:cs + 4].rearrange("p b i c -> p (b i) c"), in_=p_[:].rearrange("p c b i -> p (b i) c"), func=AF.Copy),
                         r=[(p_.name, k) for k in range(4)], w=[("Yst", cs)])
                if c % 64 == 63:
                    cb = c - 63
                    for b in range(4):
                        S.dma("sp", yc_d[b * 4096:(b + 1) * 4096, cb:cb + 64].rearrange("(i p) c -> p i c", p=128), Yst[:, b, :, :],
                              r=[("Yst", k) for k in range(0, 64, 4)], w=[("yc", b, cb)])
            if has_ctx:
                Ac = [S.sb(f"Ac{i}", [128, 384], BF16, p2) for i in range(2)]
                Ystc = S.sb("Ystc", [128, 4, 2, 64], F32, p2)
                poc = [S.ps(f"poc{i}", [128, 4, 4, 2], F32, p2) for i in range(2)]
                for c in range(256):
                    a_ = Ac[c % 2]
                    S.dma("sp", a_[:], bass.AP(Kdc.tensor, c * 512 + 1, [[1, 128], [1, 384]]), w=[a_.name])
                    p_ = poc[(c // 4) % 2]
                    for di, d in enumerate([0, -1, 1]):
                        if d >= 0:
                            J0, J1, I0, I1 = 0, 2 - d, d, 2
                        else:
                            J0, J1, I0, I1 = -d, 2, 0, 2 + d
                        S.op("pe", lambda e, d=d, J0=J0, J1=J1, I0=I0, I1=I1, a_=a_, p_=p_, c=c, di=di: e.matmul(
                            p_[:, c % 4, :, I0:I1], a_[:, 128 * (d + 1):128 * (d + 2)], ZTc[:, c, :, J0:J1], start=(di == 0), stop=(di == 2)),
                            r=[a_.name], w=[(p_.name, c % 4)] if di in (0, 2) else [])
                    if c % 4 == 3:
                        cs = (c % 64) - 3
                        S.op("act", lambda e, p_=p_, cs=cs: e.activation(out=Ystc[:, :, :, cs:cs + 4].rearrange("p b i c -> p (b i) c"), in_=p_[:].rearrange("p c b i -> p (b i) c"), func=AF.Copy),
                             r=[(p_.name, k) for k in range(4)], w=[("Ystc", cs)])
                    if c % 64 == 63:
                        cb = c - 63
                        for b in range(4):
                            S.dma("sp", yc_d[NL + b * 256:NL + (b + 1) * 256, cb:cb + 64].rearrange("(i p) c -> p i c", p=128), Ystc[:, b, :, :],
                                  r=[("Ystc", k) for k in range(0, 64, 4)], w=[("ycc", b, cb)])
            S.barrier()
        for eng in S.ENG:
            S.wait_all(eng)
        print("hyena built: ins", S.n_ins, "waits", S.n_wait)
    return nc


HY_BANDS = 16
HY_MIN_DECAY = math.log(1e-2) / 1.5
HY_MAX_DECAY = math.log(1e-2) / 0.3


def _hyena_consts(n):
    f32 = np.float32
    t = np.linspace(0.0, 1.0, n, dtype=f32)
    w = (f32(2.0 * math.pi) * np.arange(n, dtype=f32) / f32(n)).astype(f32)
    f = np.linspace(1e-4, HY_BANDS - 1, HY_BANDS, dtype=f32)
    fw = (f[None, :] * w[:, None]).astype(f32)
    z = np.concatenate([t[:, None], np.cos(fw), -np.sin(fw)], axis=-1).astype(f32)
    deltas = np.abs(np.linspace(HY_MIN_DECAY, HY_MAX_DECAY, D, dtype=f32))
    decay = np.exp(-t[:, None] * deltas[None, :]).astype(f32)
    pos = np.abs(np.arange(2 * n) - n)
    pos[0] = 0
    zc = np.ascontiguousarray(z[pos].T)
    dc = decay[pos]
    return zc, dc


def _shift_mats():
    sp = np.zeros((4, 128, 128), np.float32)
    for t in range(128):
        if t + 1 < 128:
            sp[0, t + 1, t] = 1.0
        if t - 1 >= 0:
            sp[2, t - 1, t] = 1.0
    sp[1, 0, 127] = 1.0
    sp[3, 127, 0] = 1.0
    return sp


def _block_reverse(a):
    n = a.shape[0]
    return np.ascontiguousarray(a.reshape(n // 128, 128, *a.shape[1:])[:, ::-1].reshape(a.shape))


def _condT(rows):
    r = np.stack(rows).astype(np.float32)
    return np.ascontiguousarray(r.reshape(len(rows), 16, 128).transpose(2, 1, 0))


def hyena_inputs(inp, l, xl, xc, core):
    i = l // 2
    cs = slice(core * 256, (core + 1) * 256)
    cols = np.concatenate([np.arange(core * 256, (core + 1) * 256) + k * D for k in range(3)])
    parts = [_block_reverse(xl[b]) for b in range(4)]
    if xc is not None:
        parts += [_block_reverse(xc[b]) for b in range(4)]
    d = {"xr": np.concatenate(parts, axis=0)}
    d["condT"] = _condT([inp["c"][b] for b in range(4)] + [inp["c_ctx"]])
    d["ada_w"] = inp["ada_w"][l]; d["ada_b"] = inp["ada_b"][l].reshape(1, -1)
    d["w_in"] = np.ascontiguousarray(inp["hy_w_in"][i][:, cols])
    cw = inp["hy_conv_w"][i]
    d["vecs"] = np.ascontiguousarray(np.stack([inp["hy_b_in"][i][cols], cw[0][cols], cw[1][cols], cw[2][cols], inp["hy_conv_b"][i][cols]]))
    d["fw1"] = inp["hy_f_w1"][i]
    d["fw23"] = np.ascontiguousarray(np.stack([inp["hy_f_w2"][i], inp["hy_f_w3"][i]]))
    d["fvec"] = np.ascontiguousarray(np.stack([inp["hy_f_b1"][i], inp["hy_f_b2"][i], inp["hy_f_b3"][i], inp["hy_f_freq"][i]], axis=1))
    w4 = inp["hy_f_w4"][i]
    d["fw4"] = np.ascontiguousarray(np.concatenate([w4[:, cs], w4[:, D + core * 256:D + (core + 1) * 256]], axis=1))
    d["lb"] = np.ascontiguousarray(inp["hy_long_bias"][i][cs].reshape(2, 128).T)
    zc, dc = _hyena_consts(4096)
    d["zemb"] = zc
    d["decay"] = np.ascontiguousarray(dc[:, cs].reshape(8192, 2, 128).transpose(2, 1, 0))
    if xc is not None:
        zcc, dcc = _hyena_consts(256)
        d["zembc"] = zcc
        d["decayc"] = np.ascontiguousarray(dcc[:, cs].reshape(512, 2, 128).transpose(2, 1, 0))
    d["ident"] = np.eye(128, dtype=np.float32)
    d["shm"] = _shift_mats()
    return d


def _rope_tables():
    rows = 64
    row = np.repeat(np.arange(rows, dtype=np.float32), 64)
    col = np.tile(np.arange(64, dtype=np.float32), rows)
    inv = (np.float32(10000.0) ** (-np.arange(32, dtype=np.float32) / np.float32(32))).astype(np.float32)
    ang = np.concatenate([row[:, None] * inv, col[:, None] * inv], axis=-1).astype(np.float32)
    return np.cos(ang).astype(np.float32), np.sin(ang).astype(np.float32)


_PROGS = {}


def _prog(key, fn):
    if key not in _PROGS:
        _PROGS[key] = fn()
    return _PROGS[key]


def _run(nc, in_maps):
    res = run_bass_kernel_spmd(nc, in_maps, core_ids=list(range(NCORES)))
    return res.results


def _tail_common(inp, l):
    d = {}
    d["ada_w"] = inp["ada_w"][l]; d["ada_b"] = inp["ada_b"][l].reshape(1, -1)
    d["ln"] = np.ascontiguousarray(np.stack([inp["ln1_g"][l], inp["ln1_b"][l], inp["ln2_g"][l], inp["ln2_b"][l]]))
    d["w_router"] = inp["moe_w_router"][l]; d["b_router"] = inp["moe_b_router"][l].reshape(1, -1)
    d["w_up"] = inp["moe_w_up"][l]
    d["b_upT"] = np.ascontiguousarray(inp["moe_b_up"][l].reshape(32, 12, 128).transpose(2, 0, 1).reshape(128, 32 * 12))
    d["w_down"] = inp["moe_w_down"][l]; d["b_down"] = inp["moe_b_down"][l]
    d["ident"] = np.eye(128, dtype=np.float32)
    return d


def _tok_shard(xl, xc, b, h):
    if xc is None:
        return np.ascontiguousarray(xl[b, h * 2048:(h + 1) * 2048])
    return np.ascontiguousarray(np.concatenate([xl[b, h * 2048:(h + 1) * 2048], xc[b, h * 128:(h + 1) * 128]], axis=0))


def _run_tail(inp, l, m_list, m2_list, xl, xc, wout, bout, with_ctx):
    NT = 2176 if with_ctx else 2048
    split = [(9, [384, 384, 384]), (8, [512, 512])] if with_ctx else [(8, [512, 512]), (8, [512, 512])]
    gated = m2_list is not None
    nc = _prog(("tail", NT, gated), lambda: build_tail(NT, with_ctx, split, gated=gated))
    common = _tail_common(inp, l)
    common["wout"] = wout; common["bout"] = bout.reshape(1, -1)
    maps = []
    for core in range(NCORES):
        b, h = core // 2, core % 2
        d = dict(common)
        d["m"] = m_list[core]
        if gated:
            d["m2"] = m2_list[core]
        d["x"] = _tok_shard(xl, xc if with_ctx else None, b, h)
        d["condT"] = _condT([inp["c"][b], inp["c_ctx"]])
        maps.append(d)
    res = _run(nc, maps)
    xl_n = np.empty_like(xl)
    xc_n = np.empty_like(xc) if with_ctx else None
    for core in range(NCORES):
        b, h = core // 2, core % 2
        o = res[core]["xo"]
        xl_n[b, h * 2048:(h + 1) * 2048] = o[:2048]
        if with_ctx:
            xc_n[b, h * 128:(h + 1) * 128] = o[2048:]
    return xl_n, xc_n


def _attn_layer(inp, l, xl, xc, need_ctx):
    i = l // 2
    lam_init = 0.8 - 0.6 * math.exp(-0.3 * l)
    cos, sin = _rope_tables()
    nc1 = _prog(("ain",), build_attn_in)
    maps = []
    for core in range(NCORES):
        b, h = core // 2, core % 2
        sl = slice(h * 2048, (h + 1) * 2048)
        maps.append({
            "x": _tok_shard(xl, xc, b, h),
            "condT": _condT([inp["c"][b], inp["c_ctx"]]),
            "ada_w": inp["ada_w"][l], "ada_b": inp["ada_b"][l].reshape(1, -1),
            "w_in": inp["attn_w_in"][i],
            "normw": np.ascontiguousarray(np.stack([inp["gqa_q_norm_w"][i], inp["gqa_k_norm_w"][i]])),
            "cosT": np.ascontiguousarray(cos[sl].reshape(16, 128, 64).transpose(1, 0, 2)),
            "sinT": np.ascontiguousarray(sin[sl].reshape(16, 128, 64).transpose(1, 0, 2)),
            "ident": np.eye(128, dtype=np.float32)})
    r1 = _run(nc1, maps)
    nc2 = _prog(("attn", need_ctx, l), lambda: build_attn(need_ctx, lam_init))
    lamv = np.ascontiguousarray(np.stack([inp["diff_lam_q1"][i], inp["diff_lam_k1"][i], inp["diff_lam_q2"][i], inp["diff_lam_k2"][i]]))
    subw = inp["diff_subln_w"][i].reshape(1, -1)
    maps = []
    for core in range(NCORES):
        b = core // 2
        c0, c1 = r1[2 * b], r1[2 * b + 1]
        KT_all = np.ascontiguousarray(np.concatenate([c0["KT"][:, :, 2048:], c1["KT"][:, :, 2048:], c0["KT"][:, :, :2048], c1["KT"][:, :, :2048]], axis=2))
        V_all = np.ascontiguousarray(np.concatenate([c0["V"][2048:], c1["V"][2048:], c0["V"][:2048], c1["V"][:2048]], axis=0))
        maps.append({"QT": r1[core]["QT"], "KT": KT_all, "V": V_all, "lamv": lamv, "subw": subw})
    r2 = _run(nc2, maps)
    m_list = [r2[core]["m"] for core in range(NCORES)]
    return _run_tail(inp, l, m_list, None, xl, xc, inp["attn_w_out"][i], np.zeros(D, np.float32), need_ctx)


def _hyena_layer(inp, l, xl, xc, with_ctx):
    i = l // 2
    nc1 = _prog(("hy", with_ctx), lambda: build_hyena(with_ctx))
    maps = [hyena_inputs(inp, l, xl, xc if with_ctx else None, core) for core in range(NCORES)]
    r1 = _run(nc1, maps)
    YC = np.concatenate([r1[c]["yc"] for c in range(NCORES)], axis=1)
    X0r = np.concatenate([r1[c]["x0o"] for c in range(NCORES)], axis=1)
    X0 = _block_reverse(X0r)
    def shard(A, b, h):
        parts = [A[b * 4096 + h * 2048: b * 4096 + (h + 1) * 2048]]
        if with_ctx:
            parts.append(A[16384 + b * 256 + h * 128: 16384 + b * 256 + (h + 1) * 128])
        return np.ascontiguousarray(np.concatenate(parts, axis=0))
    m_list = [shard(YC, c // 2, c % 2) for c in range(NCORES)]
    m2_list = [shard(X0, c // 2, c % 2) for c in range(NCORES)]
    return _run_tail(inp, l, m_list, m2_list, xl, xc, inp["hy_w_out"][i], inp["hy_b_out"][i], with_ctx)


def kernel(**inputs):
    inp = {k: np.asarray(v) for k, v in inputs.items()}
    xl = np.ascontiguousarray(inp["x"], dtype=np.float32)
    xc = np.ascontiguousarray(inp["ctx"], dtype=np.float32)
    xl, xc = _attn_layer(inp, 0, xl, xc, True)
    xl, xc = _hyena_layer(inp, 1, xl, xc, True)
    xl, _ = _attn_layer(inp, 2, xl, xc, False)
    xl, _ = _hyena_layer(inp, 3, xl, None, False)
    return xl
```
